# Optimizing a Trainium2 kernel written in Bass

```python
import jax, jax.numpy as jnp
from jax import lax
import numpy as np

D_MODEL = 2048
BATCH = 4
SEQ = 4096
DEPTH = 2

PLE_DIM = 256
MIX_WIDTH = D_MODEL
HGRN_WIDTH = MIX_WIDTH // 2
GLA_WIDTH = MIX_WIDTH - HGRN_WIDTH
HGRN_HEAD_DIM = 128
HGRN_HEADS = HGRN_WIDTH // HGRN_HEAD_DIM
GLA_HEADS = 4
GLA_KEY_WIDTH = GLA_WIDTH // 2
GLA_HEAD_K = GLA_KEY_WIDTH // GLA_HEADS
GLA_HEAD_V = GLA_WIDTH // GLA_HEADS
GLA_GATE_RANK = 16
GLA_GATE_TEMP = 16.0
CHUNK = 64
FFN_DIM = 5632
N_EXPERTS = 8
TOP_K = 2
EXPERT_FFN_DIM = 7168
MOE_BLOCK = 512
EPS = 1e-6
N_DENSE = (DEPTH + 1) // 2
N_MOE = DEPTH // 2
IN_SIZES = (HGRN_WIDTH, HGRN_WIDTH, HGRN_WIDTH, HGRN_WIDTH,
            GLA_KEY_WIDTH, GLA_KEY_WIDTH, GLA_WIDTH, GLA_GATE_RANK, GLA_WIDTH)
IN_COLS = sum(IN_SIZES)
IN_SPLITS = tuple(int(s) for s in np.cumsum(IN_SIZES)[:-1])

kernel_name = 'hymba_hgrn2_gla_moe_ple_trunk'


def rms_norm(x, w):
    xf = x.astype(jnp.float32)
    y = xf * lax.rsqrt(jnp.mean(xf * xf, axis=-1, keepdims=True) + EPS)
    return (y * w.astype(jnp.float32)).astype(x.dtype)


def head_rms_norm(o, w):
    H, d = o.shape[-2], o.shape[-1]
    y = o * lax.rsqrt(jnp.mean(o * o, axis=-1, keepdims=True) + EPS)
    return y * w.astype(jnp.float32).reshape(H, d)


def chunk_gated_linear_attn(q, k, v, log_a):
    B, T, H, dk = q.shape
    dv = v.shape[-1]
    n = T // CHUNK

    def to_chunks(t):
        t = t.astype(jnp.float32)
        return t.reshape(B, n, CHUNK, H, t.shape[-1]).transpose(1, 0, 3, 2, 4)

    causal = jnp.tril(jnp.ones((CHUNK, CHUNK), dtype=bool))[:, :, None]

    def step(state, inp):
        qc, kc, vc, gc = inp
        g_cum = jnp.cumsum(gc, axis=2)
        diff = g_cum[:, :, :, None, :] - g_cum[:, :, None, :, :]
        decay = jnp.exp(jnp.where(causal, diff, -jnp.inf))
        scores = jnp.einsum('bhid,bhjd,bhijd->bhij', qc, kc, decay)
        out = (jnp.einsum('bhij,bhjv->bhiv', scores, vc)
               + jnp.einsum('bhid,bhdv->bhiv', qc * jnp.exp(g_cum), state))
        g_last = g_cum[:, :, -1, :]
        k_dec = kc * jnp.exp(g_last[:, :, None, :] - g_cum)
        state = jnp.exp(g_last)[..., None] * state + jnp.einsum('bhjd,bhjv->bhdv', k_dec, vc)
        return state, out

    s0 = jnp.zeros((B, H, dk, dv), jnp.float32)
    _, out = lax.scan(step, s0, (to_chunks(q), to_chunks(k), to_chunks(v), to_chunks(log_a)))
    return out.transpose(1, 0, 3, 2, 4).reshape(B, T, H, dv)


def hybrid_mixer(hn, w_in, lb, hgrn_norm_w, gla_w2, gla_b, gla_norm_w, w_out):
    B, T, _ = hn.shape
    proj = hn @ w_in
    hq, hf, hi, hg, gq, gk, gv, glr, gg = jnp.split(proj, IN_SPLITS, axis=-1)

    lbf = lb.astype(jnp.float32)
    log_f = jnp.logaddexp(jnp.log(lbf), jnp.log1p(-lbf) + jax.nn.log_sigmoid(hf.astype(jnp.float32)))
    k_h = -jnp.expm1(log_f)
    q_h = jax.nn.silu(hq)
    o_h = chunk_gated_linear_attn(q_h.reshape(B, T, HGRN_HEADS, HGRN_HEAD_DIM),
                                  k_h.reshape(B, T, HGRN_HEADS, HGRN_HEAD_DIM),
                                  hi.reshape(B, T, HGRN_HEADS, HGRN_HEAD_DIM),
                                  log_f.reshape(B, T, HGRN_HEADS, HGRN_HEAD_DIM))
    o_h = head_rms_norm(o_h, hgrn_norm_w) * jax.nn.sigmoid(
        hg.astype(jnp.float32)).reshape(B, T, HGRN_HEADS, HGRN_HEAD_DIM)

    log_a = jax.nn.log_sigmoid((glr @ gla_w2 + gla_b).astype(jnp.float32)) / GLA_GATE_TEMP
    o_g = chunk_gated_linear_attn((gq * (GLA_HEAD_K ** -0.5)).reshape(B, T, GLA_HEADS, GLA_HEAD_K),
                                  gk.reshape(B, T, GLA_HEADS, GLA_HEAD_K),
                                  gv.reshape(B, T, GLA_HEADS, GLA_HEAD_V),
                                  log_a.reshape(B, T, GLA_HEADS, GLA_HEAD_K))
    o_g = head_rms_norm(o_g, gla_norm_w) * jax.nn.silu(
        gg.astype(jnp.float32)).reshape(B, T, GLA_HEADS, GLA_HEAD_V)

    o = jnp.concatenate([o_h.reshape(B, T, HGRN_WIDTH), o_g.reshape(B, T, GLA_WIDTH)], axis=-1)
    return o.astype(hn.dtype) @ w_out


def swiglu(x, w_gate, w_up, w_down):
    return (jax.nn.silu(x @ w_gate) * (x @ w_up)) @ w_down


def moe_swiglu(hn, router_w, w_gate, w_up, w_down):
    B, T, D = hn.shape
    tokens = hn.reshape(-1, D)
    n = tokens.shape[0]
    logits = (tokens @ router_w).astype(jnp.float32)
    top_logit, top_e = lax.top_k(logits, TOP_K)
    top_w = jax.nn.softmax(top_logit, axis=-1)
    flat_e = top_e.reshape(-1)
    flat_tok = jnp.repeat(jnp.arange(n, dtype=jnp.int32), TOP_K)
    flat_w = top_w.reshape(-1)
    n_assign = n * TOP_K
    order = jnp.argsort(flat_e)
    sorted_e = flat_e[order]
    counts = jnp.bincount(flat_e, length=N_EXPERTS)
    padded = (counts + MOE_BLOCK - 1) // MOE_BLOCK * MOE_BLOCK
    padded_end = jnp.cumsum(padded)
    padded_start = padded_end - padded
    start = jnp.cumsum(counts) - counts
    dest = padded_start[sorted_e] + jnp.arange(n_assign, dtype=jnp.int32) - start[sorted_e]
    n_blocks = -(-n_assign // MOE_BLOCK) + N_EXPERTS
    cap = n_blocks * MOE_BLOCK
    slot_tok = jnp.full((cap,), n, jnp.int32).at[dest].set(flat_tok[order])
    slot_w = jnp.zeros((cap,), jnp.float32).at[dest].set(flat_w[order])
    block_e = jnp.minimum(jnp.searchsorted(padded_end, jnp.arange(n_blocks) * MOE_BLOCK, side='right'),
                          N_EXPERTS - 1)

    def run_block(args):
        tok_ids, e = args
        xb = jnp.take(tokens, tok_ids, axis=0, mode='clip')
        return swiglu(xb, w_gate[e], w_up[e], w_down[e])

    y = lax.map(run_block, (slot_tok.reshape(n_blocks, MOE_BLOCK), block_e))
    y = y.reshape(cap, D) * slot_w[:, None].astype(y.dtype)
    out = jnp.zeros_like(tokens).at[slot_tok].add(y, mode='drop')
    return out.reshape(B, T, D)


def setup_inputs(seed: int = 0) -> dict:
    key = jax.random.key(seed)
    ks = jax.random.split(key, 24)
    f32 = jnp.float32

    def nrm(k, shape, fan_in):
        return jax.random.normal(k, shape, f32) * (fan_in ** -0.5)

    def gain(k, shape):
        return 1.0 + 0.02 * jax.random.normal(k, shape, f32)

    return {
        'x': jax.random.normal(ks[0], (BATCH, SEQ, D_MODEL), f32),
        'p': jax.random.normal(ks[1], (DEPTH, BATCH, SEQ, PLE_DIM), f32),
        'norm_mix_w': gain(ks[2], (DEPTH, D_MODEL)),
        'w_in': nrm(ks[3], (DEPTH, D_MODEL, IN_COLS), D_MODEL),
        'hgrn_lb_logits': 0.5 * jax.random.normal(ks[4], (DEPTH, HGRN_WIDTH), f32),
        'hgrn_norm_w': gain(ks[5], (DEPTH, HGRN_WIDTH)),
        'gla_gate_w2': nrm(ks[6], (DEPTH, GLA_GATE_RANK, GLA_KEY_WIDTH), GLA_GATE_RANK),
        'gla_gate_b': 0.1 * jax.random.normal(ks[7], (DEPTH, GLA_KEY_WIDTH), f32),
        'gla_norm_w': gain(ks[8], (DEPTH, GLA_WIDTH)),
        'w_out': nrm(ks[9], (DEPTH, MIX_WIDTH, D_MODEL), MIX_WIDTH),
        'norm_ffn_w': gain(ks[10], (DEPTH, D_MODEL)),
        'dense_w_gate': nrm(ks[11], (N_DENSE, D_MODEL, FFN_DIM), D_MODEL),
        'dense_w_up': nrm(ks[12], (N_DENSE, D_MODEL, FFN_DIM), D_MODEL),
        'dense_w_down': nrm(ks[13], (N_DENSE, FFN_DIM, D_MODEL), FFN_DIM),
        'moe_router': nrm(ks[14], (N_MOE, D_MODEL, N_EXPERTS), D_MODEL),
        'moe_w_gate': nrm(ks[15], (N_MOE, N_EXPERTS, D_MODEL, EXPERT_FFN_DIM), D_MODEL),
        'moe_w_up': nrm(ks[16], (N_MOE, N_EXPERTS, D_MODEL, EXPERT_FFN_DIM), D_MODEL),
        'moe_w_down': nrm(ks[17], (N_MOE, N_EXPERTS, EXPERT_FFN_DIM, D_MODEL), EXPERT_FFN_DIM),
        'norm_ple_w': gain(ks[18], (DEPTH, D_MODEL)),
        'ple_w_gate': nrm(ks[19], (DEPTH, D_MODEL, D_MODEL), D_MODEL),
        'ple_w_proj': nrm(ks[20], (DEPTH, PLE_DIM, D_MODEL), PLE_DIM),
        'final_norm_w': gain(ks[21], (D_MODEL,)),
    }


def reference(x, p, norm_mix_w, w_in, hgrn_lb_logits, hgrn_norm_w, gla_gate_w2, gla_gate_b,
              gla_norm_w, w_out, norm_ffn_w, dense_w_gate, dense_w_up, dense_w_down,
              moe_router, moe_w_gate, moe_w_up, moe_w_down, norm_ple_w, ple_w_gate,
              ple_w_proj, final_norm_w):
    lbs = jnp.cumsum(jax.nn.softmax(hgrn_lb_logits.astype(jnp.float32), axis=0), axis=0)
    lbs = lbs - lbs[0]
    h = x
    for i in range(DEPTH):
        h = h + hybrid_mixer(rms_norm(h, norm_mix_w[i]), w_in[i], lbs[i], hgrn_norm_w[i],
                             gla_gate_w2[i], gla_gate_b[i], gla_norm_w[i], w_out[i])
        hn = rms_norm(h, norm_ffn_w[i])
        j = i // 2
        if i % 2 == 0:
            h = h + swiglu(hn, dense_w_gate[j], dense_w_up[j], dense_w_down[j])
        else:
            h = h + moe_swiglu(hn, moe_router[j], moe_w_gate[j], moe_w_up[j], moe_w_down[j])
        gate = jax.nn.sigmoid(rms_norm(h, norm_ple_w[i]) @ ple_w_gate[i])
        h = h + gate * (p[i] @ ple_w_proj[i])
    return rms_norm(h, final_norm_w)
```

```python
import contextlib
import numpy as np
import concourse.bass as bass
import concourse.mybir as mybir
from concourse.bass_utils import run_bass_kernel_spmd

F32 = mybir.dt.float32
BF16 = mybir.dt.bfloat16
AF = mybir.ActivationFunctionType
ALU = mybir.AluOpType
AX = mybir.AxisListType
EPS = 1e-6
TT = 512
NST = TT // 128


class Sched:
    NDMA = 8

    def __init__(self, nc, stack):
        self.nc = nc
        self.E = {'pe': nc.tensor, 'act': nc.scalar, 'dve': nc.vector, 'pool': nc.gpsimd, 'sp': nc.sync}
        self.sem = {k: stack.enter_context(nc.semaphore("s_" + k)) for k in self.E}
        self.cnt = {k: 0 for k in self.E}
        self.dsem = {q: [stack.enter_context(nc.semaphore(f"d_{q}{i}")) for i in range(self.NDMA)]
                     for q in ('pool', 'sp')}
        self.dval = {q: [0] * self.NDMA for q in self.dsem}
        self.dn = {q: 0 for q in self.dsem}
        self.known = {k: {} for k in self.E}
        self.semobj = {}
        self.lastw = {}
        self.readers = {}
        self.ninst = 0
        self.nwait = 0

    def _tok(self, sem, val):
        self.semobj[id(sem)] = sem
        return (id(sem), val)

    def _need(self, e, toks):
        kn = self.known[e]
        best = {}
        for t in toks:
            if t is None:
                continue
            sid, v = t
            if kn.get(sid, 0) >= v:
                continue
            if best.get(sid, 0) < v:
                best[sid] = v
        for sid, v in best.items():
            self.E[e].wait_ge(self.semobj[sid], v)
            kn[sid] = v
            self.nwait += 1

    def _deps(self, reads, writes):
        toks = []
        for r in reads:
            toks.extend(self.lastw.get(r, ()))
        for w in writes:
            toks.extend(self.lastw.get(w, ()))
            for sid, v in self.readers.get(w, {}).items():
                toks.append((sid, v))
        return toks

    def _commit(self, tok, reads, writes, accum=False):
        for r in reads:
            d = self.readers.setdefault(r, {})
            if d.get(tok[0], 0) < tok[1]:
                d[tok[0]] = tok[1]
        for w in writes:
            if accum:
                self.lastw[w] = tuple(self.lastw.get(w, ())) + (tok,)
            else:
                self.lastw[w] = (tok,)
                self.readers[w] = {}

    def op(self, e, fn, reads=(), writes=()):
        psr = [r for r in reads if r.startswith('ps') and r not in writes]
        if psr:
            writes = list(writes) + psr
        toks = self._deps(reads, writes)
        if e == 'pe':
            own = id(self.sem['pe'])
            toks = [t for t in toks if t is not None and t[0] != own]
        self._need(e, toks)
        ins = fn()
        self.cnt[e] += 1
        ins.then_inc(self.sem[e], 1)
        self._commit(self._tok(self.sem[e], self.cnt[e]), reads, writes)
        self.ninst += 1

    def burst(self, fns, reads=(), writes=()):
        psr = [r for r in reads if r.startswith('ps') and r not in writes]
        if psr:
            writes = list(writes) + psr
        own = id(self.sem['pe'])
        toks = [t for t in self._deps(reads, writes) if t is not None and t[0] != own]
        self._need('pe', toks)
        for fn in fns[:-1]:
            fn()
            self.ninst += 1
        ins = fns[-1]()
        self.cnt['pe'] += 1
        ins.then_inc(self.sem['pe'], 1)
        self._commit(self._tok(self.sem['pe'], self.cnt['pe']), reads, writes)
        self.ninst += 1

    def dma(self, q, fn, reads=(), writes=(), accum=False):
        k = self.dn[q] % self.NDMA
        self.dn[q] += 1
        toks = self._deps(reads, () if accum else writes)
        if self.dval[q][k] > 0:
            toks.append(self._tok(self.dsem[q][k], self.dval[q][k]))
        self._need(q, toks)
        ins = fn()
        self.dval[q][k] += 16
        ins.then_inc(self.dsem[q][k], 16)
        self._commit(self._tok(self.dsem[q][k], self.dval[q][k]), reads, writes, accum=accum)
        self.ninst += 1

    def alias_barrier(self, src, dst):
        toks = []
        for k in src:
            toks.extend(self.lastw.get(k, ()))
            toks.extend(self.readers.get(k, {}).items())
        for d in dst:
            self.lastw[d] = tuple(self.lastw.get(d, ())) + tuple(toks)

    def wait_all(self, e, keys):
        toks = []
        for k in keys:
            toks.extend(self.lastw.get(k, ()))
        self._need(e, toks)


def make_consts():
    t = np.arange(128)
    same = (t[:, None] // 64) == (t[None, :] // 64)
    ident = np.eye(128, dtype=np.float32)
    mask = (same & (t[:, None] <= t[None, :])).astype(np.float32)
    mtri = mask.copy()
    mlast = np.stack([(t // 64 == 0), (t // 64 == 1)], 1).astype(np.float32)
    mcat = np.concatenate([mtri, mlast], 1)
    mdec = (same & (t[:, None] > t[None, :])).astype(np.float32)
    ustrict = (t[:, None] < t[None, :]).astype(np.float32)
    iota = np.tile(np.arange(256, dtype=np.float32)[None, :], (128, 1))
    return np.concatenate([ident, mask, mcat, mdec, mcat / 16.0, mdec / 16.0, ustrict, iota], 1).astype(np.float32)


C_ID, C_MASK, C_MCAT, C_MDEC, C_MCATG, C_MDECG = 0, 128, 256, 386, 514, 644
C_US, C_IOTA = 772, 900
C_TOT = 1156


def build_program(cfg, dbg=False):
    D, T, NSEQ = cfg['D'], cfg['T'], cfg['NSEQ']
    HH, GH, F0, FE, NE = cfg['HH'], cfg['GH'], cfg['F0'], cfg['FE'], cfg['NE']
    PL, R = 256, 16
    KT = D // 128
    HW, GK, GW = HH * 128, GH * 128, GH * 256
    MIXW = HW + GW
    MKT = MIXW // 128
    INC = 4 * HW + 2 * GK + GW + R + GW
    NPRE = cfg.get('NPRE', 0)
    NTILE = T // TT
    NOWN = NTILE - NPRE
    NTOK = NSEQ * T
    NTOKO = NSEQ * NOWN * TT
    assert T % TT == 0 and D % 512 == 0 and F0 % 512 == 0 and FE % 512 == 0
    o_hq, o_hf, o_hi, o_hg = 0, HW, 2 * HW, 3 * HW
    o_gq = 4 * HW
    o_gk = o_gq + GK
    o_gv = o_gk + GK
    o_glr = o_gv + GW
    o_gg = o_glr + R
    HALF = 24
    SLOTC = max(KT, 16) * 512

    nc = bass.Bass("TRN2", target_bir_lowering=False)

    def din(name, shape):
        return nc.dram_tensor(name, list(shape), F32, kind="ExternalInput").ap()

    x = din("x", [NTOK, D])
    p = din("p", [2 * NTOK, PL])
    w_in = din("w_in", [2 * D, INC])
    w_out = din("w_out", [2 * MIXW, D])
    dwg = din("dwg", [D, F0]); dwu = din("dwu", [D, F0]); dwd = din("dwd", [F0, D])
    mwr = din("mwr", [D, NE])
    mwg = din("mwg", [NE * D, FE]); mwu = din("mwu", [NE * D, FE]); mwd = din("mwd", [NE * FE, D])
    pwg = din("pwg", [2 * D, D]); pwp = din("pwp", [2 * PL, D])
    nmix = din("nmix", [2, D]); nffn = din("nffn", [2, D]); nple = din("nple", [2, D]); nfin = din("nfin", [1, D])
    lbl = din("lbl", [2, HW]); hnw = din("hnw", [2, HW]); gnw = din("gnw", [2, GW])
    gw2 = din("gw2", [2 * R, GK]); gb = din("gb", [2, GK])
    cst = din("cst", [128, C_TOT])
    out = nc.dram_tensor("out", [NTOKO, D], F32, kind="ExternalOutput").ap()
    if dbg:
        dbgo = nc.dram_tensor("dbg", [8 * NTOK, D], F32, kind="ExternalOutput").ap()

    with contextlib.ExitStack() as st:
        S = Sched(nc, st)

        def sb(name, shape, dt=F32):
            return st.enter_context(nc.sbuf_tensor(name, list(shape), dt))

        h = sb("h", [128, NST, D])
        hnT = sb("hnT", [128, KT, TT], BF16)
        tmpN = sb("tmpN", [128, D])
        wsl = [sb(f"wsl{i}", [128, SLOTC], BF16) for i in range(3)]
        actT = sb("actT", [128, max(HALF, MKT), TT], BF16)
        oT = actT
        ptile = sb("ptile", [128, NST, PL])
        pT = sb("pT", [128, PL // 128, TT], BF16)
        wfin = sb("wfin", [128, D])
        cs = sb("cs", [128, C_TOT])
        NCB, HNB, GNB = 0, 6 * KT, 6 * KT + 2 * HH
        assert GNB + 4 * GH <= 128
        allcol = sb("allcol", [128, 128]); lbcol = sb("lbcol", [128, 128])
        lbrow = sb("lbrow", [128, HW]); omlrow = sb("omlrow", [128, HW]); lbtmp = tmpN[:, 0:HW]
        omlc = sb("omlc", [128, HH])
        w2aug = sb("w2aug", [32, 2, GK], BF16)
        glrT = sb("glrT", [32, TT], BF16)
        onesh = sb("onesh", [128, 128], BF16); onesg = sb("onesg", [128, 128], BF16)
        Sh = sb("Sh", [128, 2, HH, 128]); Sg = sb("Sg", [128, 2, GH, 256])
        Sb = [sb(f"Sb{i}", [128, 256], BF16) for i in range(2)]
        qT = sb("qT", [128, TT]); kT = sb("kT", [128, TT]); gateT = sb("gateT", [128, 2, TT])
        loga = sb("loga", [128, NST, 128]); ktok = sb("ktok", [128, NST, 128])
        kdec = sb("kdec", [128, NST, 128], BF16); V = sb("V", [128, NST, 256], BF16)
        qh = sb("qh", [128, NST, 128], BF16); kh = sb("kh", [128, NST, 128], BF16)
        E1 = [sb(f"E1{i}", [128, 130]) for i in range(2)]
        E2 = [sb(f"E2{i}", [128, 128]) for i in range(2)]
        D1 = [sb(f"D1{i}", [128, 128]) for i in range(2)]
        AT = [sb(f"AT{i}", [128, 128], BF16) for i in range(2)]
        osq = [sb(f"osq{i}", [128, 2, 128], BF16) for i in range(2)]
        rstdT = [sb(f"rstdT{i}", [128, 128]) for i in range(2)]
        otmp = [sb(f"otmp{i}", [128, 128]) for i in range(2)]
        stmp = [sb(f"stmp{i}", [128, 128]) for i in range(2)]
        ss = sb("ss", [128, 8]); rs = sb("rs", [128, 8])
        gtmp = [sb(f"gtmp{i}", [128, 512]) for i in range(2)]
        lg = sb("lg", [128, NST, NE]); lg2 = sb("lg2", [128, NE]); eq1 = sb("eq1", [128, NE]); eq2 = sb("eq2", [128, NE])
        gwt = sb("gwt", [128, NST, NE]); m12 = sb("m12", [128, 4])
        CAP = cfg.get('CAP', 256); NSL = CAP // 128
        maskf = sb("maskf", [128, NST, NE]); maskb = sb("maskb", [128, NST, NE], BF16); posm = sb("posm", [128, NST, NE])
        usb = sb("usb", [128, 128], BF16); ones1b = sb("ones1b", [128, 128], BF16); identb = sb("identb", [128, 128], BF16)
        XT = hnT[:, :, 0:256]
        hnraw = actT[:, 8:24, :].rearrange("p a b -> p (a b)")[:, 0:NST * D].rearrange("p (s d) -> p s d", d=D)
        actT2 = actT[:, 0:8, :].rearrange("p a b -> p (a b)").rearrange("p (f c) -> p f c", c=256)
        assert KT >= 16 or D * NST <= 16 * 512
        ps = [st.enter_context(nc.psum_tensor(f"ps{i}", [128, 512], F32)) for i in range(7)]
        psb = st.enter_context(nc.psum_tensor("psb", [128, 1024], BF16))
        psn = [0]

        def P():
            i = psn[0] % 7
            psn[0] += 1
            return ps[i], f"ps{i}"

        ident = cs[:, C_ID:C_ID + 128]
        maskBD = cs[:, C_MASK:C_MASK + 128]

        S.dma('sp', lambda: nc.sync.dma_start(out=cs[:], in_=cst), writes=['cs'])
        S.dma('sp', lambda: nc.sync.dma_start(out=wfin[:], in_=nfin[0:1, :].partition_broadcast(128)), writes=['wfin'])
        S.op('dve', lambda: nc.vector.memset(tmpN[:, 0:256], 0.0), writes=['tmpN'])
        stg1, stg2 = tmpN[:, 0:128], tmpN[:, 128:256]
        for i, src in enumerate([nmix, nffn, nple]):
            S.dma('sp', lambda: nc.sync.dma_start(out=tmpN[2 * KT * i:2 * KT * (i + 1), 0:128], in_=src.rearrange("l (k p) -> (l k) p", p=128)),
                  writes=['tmpN'], accum=(i > 0))
        S.dma('sp', lambda: nc.sync.dma_start(out=tmpN[HNB:HNB + 2 * HH, 0:128], in_=hnw.rearrange("l (k p) -> (l k) p", p=128)), writes=['tmpN'], accum=True)
        S.dma('sp', lambda: nc.sync.dma_start(out=tmpN[GNB:GNB + 4 * GH, 0:128], in_=gnw.rearrange("l (k p) -> (l k) p", p=128)), writes=['tmpN'], accum=True)
        S.dma('sp', lambda: nc.sync.dma_start(out=tmpN[0:2 * HH, 128:256], in_=lbl.rearrange("l (k p) -> (l k) p", p=128)), writes=['tmpN'], accum=True)
        for (src_, dst_, nm) in ((stg1, allcol, 'allcol'), (stg2, lbcol, 'lbcol')):
            pt, pk = P()
            S.op('pe', lambda: nc.tensor.transpose(out=pt[:, 0:128], in_=src_, identity=ident), reads=['tmpN', 'cs'], writes=[pk])
            S.op('dve', lambda: nc.vector.tensor_copy(out=dst_[:], in_=pt[:, 0:128]), reads=[pk], writes=[nm])
        for l in range(2):
            S.dma('pool', lambda: nc.gpsimd.dma_start(out=w2aug[0:R, l, :], in_=gw2[l * R:(l + 1) * R, :]), writes=['w2aug'], accum=True)
            S.dma('pool', lambda: nc.gpsimd.dma_start(out=w2aug[R:R + 1, l, :], in_=gb[l:l + 1, :]), writes=['w2aug'], accum=True)
        S.dma('sp', lambda: nc.sync.dma_start(out=lbrow[:], in_=lbl[1:2, :].partition_broadcast(128)), writes=['lbrow'])
        S.dma('sp', lambda: nc.sync.dma_start(out=lbtmp, in_=lbl[0:1, :].partition_broadcast(128)), writes=['tmpN'])
        S.op('dve', lambda: nc.vector.tensor_tensor(out=lbtmp, in0=lbrow[:], in1=lbtmp, op=ALU.subtract),
             reads=['lbrow', 'tmpN'], writes=['tmpN'])
        S.op('act', lambda: nc.scalar.activation(out=lbrow[:], in_=lbtmp, func=AF.Sigmoid), reads=['tmpN'], writes=['lbrow'])
        S.op('dve', lambda: nc.vector.tensor_scalar(out=omlrow[:], in0=lbrow[:], scalar1=-1.0, scalar2=1.0, op0=ALU.mult, op1=ALU.add),
             reads=['lbrow'], writes=['omlrow'])
        S.op('dve', lambda: nc.vector.tensor_tensor(out=omlc[:], in0=lbcol[:, 0:HH], in1=lbcol[:, HH:2 * HH], op=ALU.subtract),
             reads=['lbcol'], writes=['omlc'])
        S.op('act', lambda: nc.scalar.activation(out=omlc[:], in_=omlc[:], func=AF.Sigmoid), reads=['omlc'], writes=['omlc'])
        S.op('dve', lambda: nc.vector.memset(onesh[:], 1.0 / 128.0), writes=['onesh'])
        S.op('dve', lambda: nc.vector.memset(onesg[:], 1.0 / 256.0), writes=['onesg'])
        S.op('dve', lambda: nc.vector.memset(glrT[:], 1.0), writes=['glrT'])
        S.op('dve', lambda: nc.vector.memset(ones1b[:], 1.0), writes=['ones1b'])
        S.op('dve', lambda: nc.vector.tensor_copy(out=usb[:], in_=cs[:, C_US:C_US + 128]), reads=['cs'], writes=['usb'])
        S.op('dve', lambda: nc.vector.tensor_copy(out=identb[:], in_=cs[:, C_ID:C_ID + 128]), reads=['cs'], writes=['identb'])

        wn = [0]

        def wload(parts):
            i = wn[0] % 3
            wn[0] += 1
            tile_, key = wsl[i], f"wsl{i}"
            first = [True]
            for (src, nk, coff, ncols, stride) in parts:
                dstv = tile_[:, 0:nk * stride].rearrange("p (k c) -> p k c", c=stride)
                srcv = src.rearrange("(k p) c -> p k c", p=128)
                for k0 in range(0, nk, 4):
                    k1 = min(nk, k0 + 4)
                    for c0 in range(0, ncols, 512):
                        c1 = min(ncols, c0 + 512)
                        S.dma('pool', lambda: nc.gpsimd.dma_start(out=dstv[:, k0:k1, coff + c0:coff + c1], in_=srcv[:, k0:k1, c0:c1]),
                              writes=[key], accum=not first[0])
                        first[0] = False
            return tile_, key

        def wview(tile_, nk, stride):
            return tile_[:, 0:nk * stride].rearrange("p (k c) -> p k c", c=stride)

        def ncix(norm_idx, k):
            return NCB + (norm_idx // 2) * 2 * KT + (norm_idx % 2) * KT + k

        def rmsnorm_T(norm_idx, keep_tok=False):
            for s_ in range(NST):
                S.op('dve', lambda: nc.vector.tensor_tensor(out=tmpN[:], in0=h[:, s_, :], in1=h[:, s_, :], op=ALU.mult),
                     reads=[f'h{s_}'], writes=['tmpN'])
                S.op('dve', lambda: nc.vector.reduce_sum(out=ss[:, 0:1], in_=tmpN[:], axis=AX.X), reads=['tmpN'], writes=['ss'])
                S.op('dve', lambda: nc.vector.tensor_scalar(out=ss[:, 1:2], in0=ss[:, 0:1], scalar1=1.0 / D, scalar2=EPS,
                                                            op0=ALU.mult, op1=ALU.add), reads=['ss'], writes=['ss'])
                S.op('act', lambda: nc.scalar.activation(out=ss[:, 2:3], in_=ss[:, 1:2], func=AF.Ln), reads=['ss'], writes=['ss'])
                S.op('act', lambda: nc.scalar.activation(out=rs[:, 0:1], in_=ss[:, 2:3], func=AF.Exp, scale=-0.5), reads=['ss'], writes=['rs'])
                S.op('act', lambda: nc.scalar.mul(out=tmpN[:], in_=h[:, s_, :], mul=rs[:, 0:1]),
                     reads=[f'h{s_}', 'rs'], writes=['tmpN'])
                if keep_tok:
                    S.op('dve', lambda: nc.vector.tensor_copy(out=hnraw[:, s_, :], in_=tmpN[:]), reads=['tmpN'], writes=['hnraw'])
                for k0 in range(0, KT, 4):
                    pt, pk = P()
                    for k in range(k0, min(k0 + 4, KT)):
                        S.op('pe', lambda: nc.tensor.transpose(out=pt[:, (k - k0) * 128:(k - k0 + 1) * 128],
                                                               in_=tmpN[:, k * 128:(k + 1) * 128], identity=ident),
                             reads=['tmpN', 'cs'], writes=[pk])
                    for k in range(k0, min(k0 + 4, KT)):
                        e = 'act' if ((k0 // 4) % 2) else 'dve'
                        if e == 'act':
                            S.op('act', lambda: nc.scalar.mul(out=hnT[:, k, s_ * 128:(s_ + 1) * 128],
                                                              in_=pt[:, (k - k0) * 128:(k - k0 + 1) * 128],
                                                              mul=allcol[:, ncix(norm_idx, k):ncix(norm_idx, k) + 1]),
                                 reads=[pk, 'allcol'], writes=['hnT'])
                        else:
                            S.op('dve', lambda: nc.vector.tensor_scalar(out=hnT[:, k, s_ * 128:(s_ + 1) * 128],
                                                                        in0=pt[:, (k - k0) * 128:(k - k0 + 1) * 128],
                                                                        scalar1=allcol[:, ncix(norm_idx, k):ncix(norm_idx, k) + 1], scalar2=None,
                                                                        op0=ALU.mult),
                                 reads=[pk, 'allcol'], writes=['hnT'])

        def proj_fm(wv, wkey, c0, m, pt, pk, ncols=TT):
            S.burst([(lambda k=k: nc.tensor.matmul(pt[0:m, 0:ncols], lhsT=wv[:, k, c0:c0 + m], rhs=hnT[:, k, 0:ncols],
                                                   start=(k == 0), stop=(k == KT - 1))) for k in range(KT)],
                    reads=[wkey, 'hnT'], writes=[pk])

        def proj_tm(wv, wkey, c0, n, s_, pt, pk):
            S.burst([(lambda k=k: nc.tensor.matmul(pt[:, 0:n], lhsT=hnT[:, k, s_ * 128:(s_ + 1) * 128], rhs=wv[:, k, c0:c0 + n],
                                                   start=(k == 0), stop=(k == KT - 1))) for k in range(KT)],
                    reads=[wkey, 'hnT'], writes=[pk])

        def chunk_attn(l, dv, Sst, skey, mc0, md0, normcol, ones_t, okt0, state_only=False):
            nb = dv // 128
            for s_ in range(NST):
                b = s_ % 2
                tsl = slice(s_ * 128, (s_ + 1) * 128)
                g1, g1k = P()
                S.op('pe', lambda: nc.tensor.matmul(g1[:, 0:130], lhsT=loga[:, s_, :], rhs=cs[:, mc0:mc0 + 130], start=True, stop=True),
                     reads=['loga', 'cs'], writes=[g1k])
                g2, g2k = P()
                S.op('pe', lambda: nc.tensor.matmul(g2[:, 0:128], lhsT=cs[:, md0:md0 + 128], rhs=loga[:, s_, :], start=True, stop=True),
                     reads=['loga', 'cs'], writes=[g2k])
                S.op('act', lambda: nc.scalar.activation(out=E1[b][:], in_=g1[:, 0:130], func=AF.Exp), reads=[g1k], writes=[f'E1{b}'])
                S.op('act', lambda: nc.scalar.activation(out=E2[b][:], in_=g1[:, 0:128], func=AF.Exp, scale=-1.0), reads=[g1k], writes=[f'E2{b}'])
                S.op('act', lambda: nc.scalar.activation(out=D1[b][:], in_=g2[:, 0:128], func=AF.Exp), reads=[g2k], writes=[f'D1{b}'])
                S.op('dve', lambda: nc.vector.tensor_tensor(out=kdec[:, s_, :], in0=ktok[:, s_, :], in1=D1[b][:], op=ALU.mult),
                     reads=['ktok', f'D1{b}'], writes=[f'kdec{s_}'])
                if state_only:
                    for c in range(2):
                        u_, uk = P()
                        S.op('pe', lambda: nc.tensor.matmul(u_[:, 0:dv], lhsT=kdec[c * 64:(c + 1) * 64, s_, :], rhs=V[c * 64:(c + 1) * 64, s_, 0:dv],
                                                            start=True, stop=True),
                             reads=[f'kdec{s_}', 'V'], writes=[uk])
                        S.op('dve', lambda: nc.vector.scalar_tensor_tensor(out=Sst, in0=Sst, scalar=E1[b][:, 128 + c:129 + c], in1=u_[:, 0:dv],
                                                                           op0=ALU.mult, op1=ALU.add),
                             reads=[skey, f'E1{b}', uk], writes=[skey])
                    continue
                S.op('dve', lambda: nc.vector.tensor_tensor(out=qh[:, s_, :], in0=qT[:, tsl], in1=E1[b][:, 0:128], op=ALU.mult),
                     reads=['qT', f'E1{b}'], writes=[f'qh{s_}'])
                S.op('dve', lambda: nc.vector.tensor_tensor(out=kh[:, s_, :], in0=kT[:, tsl], in1=E2[b][:], op=ALU.mult),
                     reads=['kT', f'E2{b}'], writes=[f'kh{s_}'])
                sc, sck = P()
                S.op('pe', lambda: nc.tensor.matmul(sc[:, 0:128], lhsT=kh[:, s_, :], rhs=qh[:, s_, :], start=True, stop=True),
                     reads=[f'kh{s_}', f'qh{s_}'], writes=[sck])
                S.op('dve', lambda: nc.vector.tensor_tensor(out=AT[b][:], in0=sc[:, 0:128], in1=maskBD, op=ALU.mult),
                     reads=[sck, 'cs'], writes=[f'AT{b}'])
                obl = [P() for _ in range(nb)]
                for blk in range(nb):
                    S.op('pe', lambda: nc.tensor.matmul(obl[blk][0][:, 0:128], lhsT=V[:, s_, blk * 128:(blk + 1) * 128], rhs=AT[b][:], start=True, stop=False),
                         reads=['V', f'AT{b}'], writes=[obl[blk][1]])
                for c in range(2):
                    sbt, sbk = Sb[c], f"Sb{c}"
                    S.op('act', lambda: nc.scalar.copy(out=sbt[:, 0:dv], in_=Sst), reads=[skey], writes=[sbk])
                    for blk in range(nb):
                        S.op('pe', lambda: nc.tensor.matmul(obl[blk][0][:, c * 64:(c + 1) * 64],
                                                            lhsT=sbt[:, blk * 128:(blk + 1) * 128], rhs=qh[:, s_, c * 64:(c + 1) * 64],
                                                            start=False, stop=(c == 1)),
                             reads=[sbk, f'qh{s_}'], writes=[obl[blk][1]])
                    u_, uk = P()
                    S.op('pe', lambda: nc.tensor.matmul(u_[:, 0:dv], lhsT=kdec[c * 64:(c + 1) * 64, s_, :], rhs=V[c * 64:(c + 1) * 64, s_, 0:dv],
                                                        start=True, stop=True),
                         reads=[f'kdec{s_}', 'V'], writes=[uk])
                    S.op('dve', lambda: nc.vector.scalar_tensor_tensor(out=Sst, in0=Sst, scalar=E1[b][:, 128 + c:129 + c], in1=u_[:, 0:dv],
                                                                       op0=ALU.mult, op1=ALU.add),
                         reads=[skey, f'E1{b}', uk], writes=[skey])
                for blk in range(nb):
                    S.op('act', lambda: nc.scalar.activation(out=osq[b][:, blk, :], in_=obl[blk][0][:, 0:128], func=AF.Square),
                         reads=[obl[blk][1]], writes=[f'osq{b}'])
                n_, nk_ = P()
                for blk in range(nb):
                    S.op('pe', lambda: nc.tensor.matmul(n_[:, 0:128], lhsT=ones_t[:], rhs=osq[b][:, blk, :], start=(blk == 0), stop=(blk == nb - 1)),
                         reads=[f'osq{b}', 'ones'], writes=[nk_])
                S.op('dve', lambda: nc.vector.tensor_scalar(out=rstdT[b][:], in0=n_[:, 0:128], scalar1=EPS, scalar2=None, op0=ALU.add),
                     reads=[nk_], writes=[f'rstdT{b}'])
                S.op('act', lambda: nc.scalar.activation(out=rstdT[b][:], in_=rstdT[b][:], func=AF.Ln), reads=[f'rstdT{b}'], writes=[f'rstdT{b}'])
                S.op('act', lambda: nc.scalar.activation(out=rstdT[b][:], in_=rstdT[b][:], func=AF.Exp, scale=-0.5), reads=[f'rstdT{b}'], writes=[f'rstdT{b}'])
                for blk in range(nb):
                    S.op('dve', lambda: nc.vector.scalar_tensor_tensor(out=otmp[b][:], in0=obl[blk][0][:, 0:128],
                                                                       scalar=normcol[:, blk:blk + 1], in1=rstdT[b][:], op0=ALU.mult, op1=ALU.mult),
                         reads=[obl[blk][1], f'rstdT{b}', 'allcol'], writes=[f'otmp{b}'])
                    S.op('dve', lambda: nc.vector.tensor_tensor(out=oT[:, okt0 + blk, tsl], in0=otmp[b][:], in1=gateT[:, blk, tsl], op=ALU.mult),
                         reads=[f'otmp{b}', 'gateT'], writes=['oT'])

        MS = cfg.get('mixsub', 99)

        def mixer(l, state_only=False):
            so = state_only
            S.alias_barrier(['actT', 'hnraw'], ['oT'])
            rmsnorm_T(0 + l)
            for hd in range(HH):
                wt, wk = wload([(w_in[l * D:(l + 1) * D, o + hd * 128: o + (hd + 1) * 128], KT, j * 128, 128, 512)
                                for j, o in enumerate([o_hq, o_hf, o_hi, o_hg]) if (not so) or j in (1, 2)])
                wv = wview(wt, KT, 512)
                if not so:
                    pa, pak = P()
                    proj_fm(wv, wk, 0, 128, pa, pak)
                    S.op('act', lambda: nc.scalar.activation(out=qT[:], in_=pa[:, :], func=AF.Silu), reads=[pak], writes=['qT'])
                    pb, pbk = P()
                    proj_fm(wv, wk, 128, 128, pb, pbk)
                    S.op('act', lambda: nc.scalar.activation(out=kT[:], in_=pb[:, :], func=AF.Sigmoid, scale=-1.0), reads=[pbk], writes=['kT'])
                    if l == 1:
                        S.op('dve', lambda: nc.vector.tensor_scalar(out=kT[:], in0=kT[:], scalar1=omlc[:, hd:hd + 1], scalar2=None, op0=ALU.mult),
                             reads=['kT', 'omlc'], writes=['kT'])
                    pc, pck = P()
                    proj_fm(wv, wk, 384, 128, pc, pck)
                    S.op('act', lambda: nc.scalar.activation(out=gateT[:, 0, :], in_=pc[:, :], func=AF.Sigmoid), reads=[pck], writes=['gateT'])
                for s_ in range(NST):
                    b = s_ % 2
                    pd, pdk = P()
                    proj_tm(wv, wk, 128, 256, s_, pd, pdk)
                    S.op('act', lambda: nc.scalar.activation(out=stmp[b][:], in_=pd[:, 0:128], func=AF.Sigmoid), reads=[pdk], writes=[f'stmp{b}'])
                    if l == 1:
                        hs = slice(hd * 128, (hd + 1) * 128)
                        S.op('dve', lambda: nc.vector.tensor_tensor(out=stmp[b][:], in0=stmp[b][:], in1=omlrow[:, hs], op=ALU.mult),
                             reads=[f'stmp{b}', 'omlrow'], writes=[f'stmp{b}'])
                        S.op('dve', lambda: nc.vector.tensor_tensor(out=stmp[b][:], in0=stmp[b][:], in1=lbrow[:, hs], op=ALU.add),
                             reads=[f'stmp{b}', 'lbrow'], writes=[f'stmp{b}'])
                    S.op('act', lambda: nc.scalar.activation(out=loga[:, s_, :], in_=stmp[b][:], func=AF.Ln), reads=[f'stmp{b}'], writes=['loga'])
                    S.op('dve', lambda: nc.vector.tensor_scalar(out=ktok[:, s_, :], in0=stmp[b][:], scalar1=-1.0, scalar2=1.0, op0=ALU.mult, op1=ALU.add),
                         reads=[f'stmp{b}'], writes=['ktok'])
                    S.op('act', lambda: nc.scalar.copy(out=V[:, s_, 0:128], in_=pd[:, 128:256]), reads=[pdk], writes=['V'])
                if MS >= 2:
                  chunk_attn(l, 128, Sh[:, l, hd, :], f"Sh{l}_{hd}", C_MCAT, C_MDEC, allcol[:, HNB + l * HH + hd:HNB + l * HH + hd + 1], onesh, hd, state_only=so)
            if MS < 3:
                return
            wt, wk = wload([(w_in[l * D:(l + 1) * D, o_glr:o_glr + R], KT, 0, R, 512)])
            wv = wview(wt, KT, 512)
            pg, pgk = P()
            proj_fm(wv, wk, 0, R, pg, pgk)
            S.op('act', lambda: nc.scalar.copy(out=glrT[0:R, :], in_=pg[0:R, :]), reads=[pgk], writes=['glrT'])
            for gh in range(GH if MS >= 4 else 0):
                gparts = [(w_in[l * D:(l + 1) * D, o_gq + gh * 128: o_gq + (gh + 1) * 128], KT, 0, 128, 512),
                          (w_in[l * D:(l + 1) * D, o_gk + gh * 128: o_gk + (gh + 1) * 128], KT, 128, 128, 512),
                          (w_in[l * D:(l + 1) * D, o_gv + gh * 256: o_gv + (gh + 1) * 256], KT, 256, 256, 512)]
                wt, wk = wload(gparts[1:] if so else gparts)
                wv = wview(wt, KT, 512)
                if not so:
                    wt2, wk2 = wload([(w_in[l * D:(l + 1) * D, o_gg + gh * 256: o_gg + (gh + 1) * 256], KT, 0, 256, 512)])
                    wv2 = wview(wt2, KT, 512)
                    pa, pak = P()
                    proj_fm(wv, wk, 0, 128, pa, pak)
                    S.op('act', lambda: nc.scalar.mul(out=qT[:], in_=pa[:, :], mul=float(128 ** -0.5)), reads=[pak], writes=['qT'])
                    pb, pbk = P()
                    proj_fm(wv, wk, 128, 128, pb, pbk)
                    S.op('act', lambda: nc.scalar.copy(out=kT[:], in_=pb[:, :]), reads=[pbk], writes=['kT'])
                    for blk in range(2):
                        pc, pck = P()
                        proj_fm(wv2, wk2, blk * 128, 128, pc, pck)
                        S.op('act', lambda: nc.scalar.activation(out=gateT[:, blk, :], in_=pc[:, :], func=AF.Silu), reads=[pck], writes=['gateT'])
                for s_ in range(NST):
                    b = s_ % 2
                    pd, pdk = P()
                    proj_tm(wv, wk, 128, 384, s_, pd, pdk)
                    S.op('act', lambda: nc.scalar.copy(out=ktok[:, s_, :], in_=pd[:, 0:128]), reads=[pdk], writes=['ktok'])
                    S.op('act', lambda: nc.scalar.copy(out=V[:, s_, :], in_=pd[:, 128:384]), reads=[pdk], writes=['V'])
                    pz, pzk = P()
                    S.op('pe', lambda: nc.tensor.matmul(pz[:, 0:128], lhsT=glrT[0:R + 1, s_ * 128:(s_ + 1) * 128],
                                                        rhs=w2aug[0:R + 1, l, gh * 128:(gh + 1) * 128], start=True, stop=True),
                         reads=['glrT', 'w2aug'], writes=[pzk])
                    S.op('act', lambda: nc.scalar.activation(out=stmp[b][:], in_=pz[:, 0:128], func=AF.Sigmoid), reads=[pzk], writes=[f'stmp{b}'])
                    S.op('act', lambda: nc.scalar.activation(out=loga[:, s_, :], in_=stmp[b][:], func=AF.Ln), reads=[f'stmp{b}'], writes=['loga'])
                chunk_attn(l, 256, Sg[:, l, gh, :], f"Sg{l}_{gh}", C_MCATG, C_MDECG, allcol[:, GNB + l * 2 * GH + 2 * gh:GNB + l * 2 * GH + 2 * gh + 2], onesg, HH + 2 * gh, state_only=so)
            for dc in range(D // 512 if (MS >= 5 and not so) else 0):
                wt, wk = wload([(w_out[l * MIXW:(l + 1) * MIXW, dc * 512:(dc + 1) * 512], MKT, 0, 512, 512)])
                wv = wview(wt, MKT, 512)
                for s_ in range(NST):
                    po, pok = P()
                    S.burst([(lambda k=k: nc.tensor.matmul(po[:, :], lhsT=oT[:, k, s_ * 128:(s_ + 1) * 128], rhs=wv[:, k, :],
                                                           start=(k == 0), stop=(k == MKT - 1))) for k in range(MKT)],
                            reads=['oT', wk], writes=[pok])
                    S.op('dve', lambda: nc.vector.tensor_tensor(out=h[:, s_, dc * 512:(dc + 1) * 512], in0=h[:, s_, dc * 512:(dc + 1) * 512],
                                                                in1=po[:, :], op=ALU.add),
                         reads=[pok, f'h{s_}'], writes=[f'h{s_}'])
            S.alias_barrier(['oT'], ['actT', 'hnraw'])

        def swiglu(wg, wu, wd, F, gate_col=None):
            NG = F // 512
            groups = list(range(NG))
            halves = [groups[i:i + HALF // 4] for i in range(0, NG, HALF // 4)]
            for hv in halves:
                nft = len(hv) * 4
                for gi, g in enumerate(hv):
                    wtg, wkg = wload([(wg[:, g * 512:(g + 1) * 512], KT, 0, 512, 512)])
                    wtu, wku = wload([(wu[:, g * 512:(g + 1) * 512], KT, 0, 512, 512)])
                    wvg, wvu = wview(wtg, KT, 512), wview(wtu, KT, 512)
                    for j in range(4):
                        b = j % 2
                        pg_, pgk_ = P()
                        proj_fm(wvg, wkg, j * 128, 128, pg_, pgk_)
                        pu_, puk_ = P()
                        proj_fm(wvu, wku, j * 128, 128, pu_, puk_)
                        S.op('act', lambda: nc.scalar.activation(out=gtmp[b][:], in_=pg_[:, :], func=AF.Silu), reads=[pgk_], writes=[f'gtmp{b}'])
                        S.op('dve', lambda: nc.vector.tensor_tensor(out=actT[:, gi * 4 + j, :], in0=gtmp[b][:], in1=pu_[:, :], op=ALU.mult),
                             reads=[f'gtmp{b}', puk_], writes=['actT'])
                for dc in range(D // 512):
                    accs = [P() for _ in range(NST)]
                    for f0 in range(0, nft, 16):
                        nf = min(16, nft - f0)
                        r0 = (hv[0] * 4 + f0) * 128
                        wt, wk = wload([(wd[r0:r0 + nf * 128, dc * 512:(dc + 1) * 512], nf, 0, 512, 512)])
                        wv = wview(wt, nf, 512)
                        S.burst([(lambda j=j, s_=s_: nc.tensor.matmul(accs[s_][0][:, :], lhsT=actT[:, f0 + j, s_ * 128:(s_ + 1) * 128], rhs=wv[:, j, :],
                                                                      start=(f0 + j == 0), stop=(f0 + j == nft - 1)))
                                 for j in range(nf) for s_ in range(NST)],
                                reads=['actT', wk], writes=[a[1] for a in accs])
                    for s_ in range(NST):
                        hsl = h[:, s_, dc * 512:(dc + 1) * 512]
                        if gate_col is None:
                            S.op('dve', lambda: nc.vector.tensor_tensor(out=hsl, in0=hsl, in1=accs[s_][0][:, :], op=ALU.add),
                                 reads=[accs[s_][1], f'h{s_}'], writes=[f'h{s_}'])
                        else:
                            S.op('dve', lambda: nc.vector.scalar_tensor_tensor(out=hsl, in0=accs[s_][0][:, :], scalar=gwt[:, s_, gate_col:gate_col + 1],
                                                                               in1=hsl, op0=ALU.mult, op1=ALU.add),
                                 reads=[accs[s_][1], f'h{s_}', 'gwt'], writes=[f'h{s_}'])

        def moe_sparse():
            S.alias_barrier(['hnT'], ['XT', 'Pe', 'PTe', 'Ydc0', 'Ydc1'])
            for s_ in range(NST):
                S.op('dve', lambda: nc.vector.tensor_scalar(out=maskf[:, s_, :], in0=gwt[:, s_, :], scalar1=0.0, scalar2=None, op0=ALU.is_gt),
                     reads=['gwt'], writes=['maskf'])
                S.op('dve', lambda: nc.vector.tensor_copy(out=maskb[:, s_, :], in_=maskf[:, s_, :]), reads=['maskf'], writes=['maskb'])
            for s_ in range(NST):
                pp, ppk = P()
                for s2 in range(s_):
                    S.op('pe', lambda: nc.tensor.matmul(pp[:, 0:NE], lhsT=ones1b[:], rhs=maskb[:, s2, :], start=(s2 == 0), stop=False),
                         reads=['ones1b', 'maskb'], writes=[ppk])
                S.op('pe', lambda: nc.tensor.matmul(pp[:, 0:NE], lhsT=usb[:], rhs=maskb[:, s_, :], start=(s_ == 0), stop=True),
                     reads=['usb', 'maskb'], writes=[ppk])
                S.op('dve', lambda: nc.vector.scalar_tensor_tensor(out=posm[:, s_, :], in0=pp[:, 0:NE], scalar=1.0, in1=maskf[:, s_, :], op0=ALU.add, op1=ALU.mult),
                     reads=[ppk, 'maskf'], writes=['posm'])
                S.op('dve', lambda: nc.vector.tensor_scalar(out=posm[:, s_, :], in0=posm[:, s_, :], scalar1=-1.0, scalar2=None, op0=ALU.add),
                     reads=['posm'], writes=['posm'])
            NG = FE // 512
            GP = 4
            for e in range(NE):
                for s_ in range(NST):
                    S.op('dve', lambda: nc.vector.tensor_scalar(out=hnT[:, s_, 256:256 + CAP], in0=cs[:, C_IOTA:C_IOTA + CAP], scalar1=posm[:, s_, e:e + 1],
                                                                scalar2=None, op0=ALU.is_equal),
                         reads=['cs', 'posm'], writes=['Pe'])
                for s_ in range(NST):
                    for t2 in range(NSL):
                        r = t2 * NST + s_
                        S.op('pe', lambda: nc.tensor.transpose(out=psb[:, r * 128:(r + 1) * 128], in_=hnT[:, s_, 256 + t2 * 128:256 + (t2 + 1) * 128], identity=identb[:]),
                             reads=['Pe', 'identb'], writes=['psb'])
                for t2 in range(NSL):
                    for hf in range(2):
                        S.op('act', lambda: nc.scalar.copy(out=hnT[:, 4 + t2 * 2 + hf, 256:512], in_=psb[:, (t2 * NST + 2 * hf) * 128:(t2 * NST + 2 * hf + 2) * 128]),
                             reads=['psb'], writes=['PTe'])
                for k0 in range(0, KT, 2):
                    pt, pk = P()
                    S.burst([(lambda k=k, s_=s_: nc.tensor.matmul(pt[:, (k - k0) * 256:(k - k0) * 256 + CAP], lhsT=hnraw[:, s_, k * 128:(k + 1) * 128],
                                                                  rhs=hnT[:, s_, 256:256 + CAP], start=(s_ == 0), stop=(s_ == NST - 1)))
                             for k in range(k0, min(k0 + 2, KT)) for s_ in range(NST)],
                            reads=['hnraw', 'Pe'], writes=[pk])
                    for k in range(k0, min(k0 + 2, KT)):
                        eng = 'act' if ((k0 // 2) % 2) else 'dve'
                        src_ = pt[:, (k - k0) * 256:(k - k0) * 256 + CAP]
                        col = allcol[:, ncix(3, k):ncix(3, k) + 1]
                        if eng == 'act':
                            S.op('act', lambda: nc.scalar.mul(out=XT[:, k, 0:CAP], in_=src_, mul=col), reads=[pk, 'allcol'], writes=['XT'])
                        else:
                            S.op('dve', lambda: nc.vector.tensor_scalar(out=XT[:, k, 0:CAP], in0=src_, scalar1=col, scalar2=None, op0=ALU.mult),
                                 reads=[pk, 'allcol'], writes=['XT'])
                wg_e, wu_e, wd_e = mwg[e * D:(e + 1) * D, :], mwu[e * D:(e + 1) * D, :], mwd[e * FE:(e + 1) * FE, :]
                for g0 in range(0, NG, GP):
                    gs = list(range(g0, min(NG, g0 + GP)))
                    nft = len(gs) * 4
                    for gi, g in enumerate(gs):
                        wtg, wkg = wload([(wg_e[:, g * 512:(g + 1) * 512], KT, 0, 512, 512)])
                        wtu, wku = wload([(wu_e[:, g * 512:(g + 1) * 512], KT, 0, 512, 512)])
                        wvg, wvu = wview(wtg, KT, 512), wview(wtu, KT, 512)
                        for j in range(4):
                            b = j % 2
                            pg_, pgk_ = P()
                            pu_, puk_ = P()
                            S.burst([(lambda k=k: nc.tensor.matmul(pg_[:, 0:CAP], lhsT=wvg[:, k, j * 128:(j + 1) * 128], rhs=XT[:, k, 0:CAP],
                                                                   start=(k == 0), stop=(k == KT - 1))) for k in range(KT)], reads=[wkg, 'XT'], writes=[pgk_])
                            S.burst([(lambda k=k: nc.tensor.matmul(pu_[:, 0:CAP], lhsT=wvu[:, k, j * 128:(j + 1) * 128], rhs=XT[:, k, 0:CAP],
                                                                   start=(k == 0), stop=(k == KT - 1))) for k in range(KT)], reads=[wku, 'XT'], writes=[puk_])
                            S.op('act', lambda: nc.scalar.activation(out=gtmp[b][:, 0:CAP], in_=pg_[:, 0:CAP], func=AF.Silu), reads=[pgk_], writes=[f'gtmp{b}'])
                            S.op('dve', lambda: nc.vector.tensor_tensor(out=actT2[:, gi * 4 + j, 0:CAP], in0=gtmp[b][:, 0:CAP], in1=pu_[:, 0:CAP], op=ALU.mult),
                                 reads=[f'gtmp{b}', puk_], writes=['actT'])
                    for dc in range(D // 512):
                        yb = dc % 2
                        accs = [P() for _ in range(NSL)]
                        r0 = gs[0] * 4 * 128
                        wt, wk = wload([(wd_e[r0:r0 + nft * 128, dc * 512:(dc + 1) * 512], nft, 0, 512, 512)])
                        wv = wview(wt, nft, 512)
                        S.burst([(lambda f=f, t2=t2: nc.tensor.matmul(accs[t2][0][:, :], lhsT=actT2[:, f, t2 * 128:(t2 + 1) * 128], rhs=wv[:, f, :],
                                                                      start=(f == 0), stop=(f == nft - 1))) for f in range(nft) for t2 in range(NSL)],
                                reads=['actT', wk], writes=[a[1] for a in accs])
                        for t2 in range(NSL):
                            for hf in range(2):
                                S.op('act', lambda: nc.scalar.copy(out=hnT[:, 8 + yb * 4 + t2 * 2 + hf, 256:512], in_=accs[t2][0][:, hf * 256:(hf + 1) * 256]),
                                     reads=[accs[t2][1]], writes=[f'Ydc{yb}'])
                        for s_ in range(NST):
                            po, pok = P()
                            for hf in range(2):
                                for t2 in range(NSL):
                                    S.op('pe', lambda: nc.tensor.matmul(po[:, hf * 256:(hf + 1) * 256],
                                                                        lhsT=hnT[:, 4 + t2 * 2 + s_ // 2, 256 + (s_ % 2) * 128:256 + (s_ % 2 + 1) * 128],
                                                                        rhs=hnT[:, 8 + yb * 4 + t2 * 2 + hf, 256:512], start=(t2 == 0), stop=(t2 == NSL - 1)),
                                         reads=['PTe', f'Ydc{yb}'], writes=[pok])
                            hsl = h[:, s_, dc * 512:(dc + 1) * 512]
                            S.op('dve', lambda: nc.vector.scalar_tensor_tensor(out=hsl, in0=po[:, :], scalar=gwt[:, s_, e:e + 1], in1=hsl, op0=ALU.mult, op1=ALU.add),
                                 reads=[pok, f'h{s_}', 'gwt'], writes=[f'h{s_}'])
            S.alias_barrier(['XT', 'Pe', 'PTe', 'Ydc0', 'Ydc1'], ['hnT'])

        def router():
            wtr, wkr = wload([(mwr, KT, 0, NE, 512)])
            wr = wview(wtr, KT, 512)
            for s_ in range(NST):
                pr, prk = P()
                S.burst([(lambda k=k: nc.tensor.matmul(pr[:, 0:NE], lhsT=hnT[:, k, s_ * 128:(s_ + 1) * 128], rhs=wr[:, k, 0:NE],
                                                       start=(k == 0), stop=(k == KT - 1))) for k in range(KT)],
                        reads=['hnT', wkr], writes=[prk])
                S.op('dve', lambda: nc.vector.tensor_copy(out=lg[:, s_, :], in_=pr[:, 0:NE]), reads=[prk], writes=['lg'])
                S.op('dve', lambda: nc.vector.reduce_max(out=m12[:, 0:1], in_=lg[:, s_, :], axis=AX.X), reads=['lg'], writes=['m12'])
                S.op('dve', lambda: nc.vector.tensor_scalar(out=eq1[:], in0=lg[:, s_, :], scalar1=m12[:, 0:1], scalar2=None, op0=ALU.is_equal),
                     reads=['lg', 'm12'], writes=['eq1'])
                S.op('dve', lambda: nc.vector.scalar_tensor_tensor(out=lg2[:], in0=eq1[:], scalar=-1e30, in1=lg[:, s_, :], op0=ALU.mult, op1=ALU.add),
                     reads=['eq1', 'lg'], writes=['lg2'])
                S.op('dve', lambda: nc.vector.reduce_max(out=m12[:, 1:2], in_=lg2[:], axis=AX.X), reads=['lg2'], writes=['m12'])
                S.op('dve', lambda: nc.vector.tensor_scalar(out=eq2[:], in0=lg2[:], scalar1=m12[:, 1:2], scalar2=None, op0=ALU.is_equal),
                     reads=['lg2', 'm12'], writes=['eq2'])
                S.op('dve', lambda: nc.vector.tensor_tensor(out=m12[:, 2:3], in0=m12[:, 0:1], in1=m12[:, 1:2], op=ALU.subtract),
                     reads=['m12'], writes=['m12'])
                S.op('act', lambda: nc.scalar.activation(out=m12[:, 2:3], in_=m12[:, 2:3], func=AF.Sigmoid), reads=['m12'], writes=['m12'])
                S.op('dve', lambda: nc.vector.tensor_scalar(out=m12[:, 3:4], in0=m12[:, 2:3], scalar1=-1.0, scalar2=1.0, op0=ALU.mult, op1=ALU.add),
                     reads=['m12'], writes=['m12'])
                S.op('dve', lambda: nc.vector.tensor_scalar(out=eq1[:], in0=eq1[:], scalar1=m12[:, 2:3], scalar2=None, op0=ALU.mult),
                     reads=['eq1', 'm12'], writes=['eq1'])
                S.op('dve', lambda: nc.vector.scalar_tensor_tensor(out=gwt[:, s_, :], in0=eq2[:], scalar=m12[:, 3:4], in1=eq1[:], op0=ALU.mult, op1=ALU.add),
                     reads=['eq1', 'eq2', 'm12'], writes=['gwt'])

        def ple(l, tok0):
            rmsnorm_T(4 + l)
            S.dma('sp', lambda: nc.sync.dma_start(out=ptile[:], in_=p[l * NTOK + tok0: l * NTOK + tok0 + TT, :].rearrange("(s p) c -> p s c", p=128)),
                  writes=['ptile'])
            for s_ in range(NST):
                pt, pk = P()
                for k2 in range(PL // 128):
                    S.op('pe', lambda: nc.tensor.transpose(out=pt[:, k2 * 128:(k2 + 1) * 128], in_=ptile[:, s_, k2 * 128:(k2 + 1) * 128], identity=ident),
                         reads=['ptile', 'cs'], writes=[pk])
                S.op('act', lambda: nc.scalar.copy(out=pT[:, :, s_ * 128:(s_ + 1) * 128],
                                                   in_=pt[:, 0:PL].rearrange("p (k c) -> p k c", c=128)), reads=[pk], writes=['pT'])
            for dc in range(D // 512):
                wtp, wkp = wload([(pwp[l * PL:(l + 1) * PL, dc * 512:(dc + 1) * 512], PL // 128, 0, 512, 512)])
                wvp = wview(wtp, PL // 128, 512)
                wt, wk = wload([(pwg[l * D:(l + 1) * D, dc * 512:(dc + 1) * 512], KT, 0, 512, 512)])
                wv = wview(wt, KT, 512)
                for s_ in range(NST):
                    b = s_ % 2
                    pa, pak = P()
                    proj_tm(wv, wk, 0, 512, s_, pa, pak)
                    pb, pbk = P()
                    for k2 in range(PL // 128):
                        S.op('pe', lambda: nc.tensor.matmul(pb[:, :], lhsT=pT[:, k2, s_ * 128:(s_ + 1) * 128], rhs=wvp[:, k2, :],
                                                            start=(k2 == 0), stop=(k2 == PL // 128 - 1)),
                             reads=['pT', wkp], writes=[pbk])
                    S.op('act', lambda: nc.scalar.activation(out=gtmp[b][:], in_=pa[:, :], func=AF.Sigmoid), reads=[pak], writes=[f'gtmp{b}'])
                    S.op('dve', lambda: nc.vector.tensor_tensor(out=gtmp[b][:], in0=gtmp[b][:], in1=pb[:, :], op=ALU.mult),
                         reads=[f'gtmp{b}', pbk], writes=[f'gtmp{b}'])
                    hsl = h[:, s_, dc * 512:(dc + 1) * 512]
                    S.op('dve', lambda: nc.vector.tensor_tensor(out=hsl, in0=hsl, in1=gtmp[b][:], op=ALU.add),
                         reads=[f'gtmp{b}', f'h{s_}'], writes=[f'h{s_}'])

        def dump(idx, tok0):
            if not dbg:
                return
            for s_ in range(NST):
                S.dma('sp', lambda: nc.sync.dma_start(out=dbgo[idx * NTOK + tok0 + s_ * 128: idx * NTOK + tok0 + (s_ + 1) * 128, :], in_=h[:, s_, :]),
                      reads=[f'h{s_}'], writes=['dbgo'], accum=True)

        for sq in range(NSEQ):
            for l in range(2):
                for hd in range(HH):
                    S.op('dve', lambda: nc.vector.memset(Sh[:, l, hd, :], 0.0), writes=[f"Sh{l}_{hd}"])
                for gh in range(GH):
                    S.op('dve', lambda: nc.vector.memset(Sg[:, l, gh, :], 0.0), writes=[f"Sg{l}_{gh}"])
            for ti in range(NTILE):
                tok0 = sq * T + ti * TT
                pre = ti < NPRE
                otok0 = (sq * NOWN + (ti - NPRE)) * TT
                for s_ in range(NST):
                    S.dma('sp', lambda: nc.sync.dma_start(out=h[:, s_, :], in_=x[tok0 + s_ * 128: tok0 + (s_ + 1) * 128, :]), writes=[f'h{s_}'])
                for l in range(2):
                    if pre and l == 1:
                        mixer(1, state_only=True)
                        break
                    mixer(l)
                    dump(3 * l + 0, tok0)
                    if l == 0:
                        rmsnorm_T(2)
                        swiglu(dwg, dwu, dwd, F0)
                    elif cfg.get('dense_moe', False):
                        rmsnorm_T(3)
                        router()
                        for e in range(NE):
                            swiglu(mwg[e * D:(e + 1) * D, :], mwu[e * D:(e + 1) * D, :], mwd[e * FE:(e + 1) * FE, :], FE, gate_col=e)
                    else:
                        rmsnorm_T(3, keep_tok=True)
                        router()
                        moe_sparse()
                    dump(3 * l + 1, tok0)
                    ple(l, tok0)
                    dump(3 * l + 2, tok0)
                if pre:
                    continue
                for s_ in range(NST):
                    S.op('dve', lambda: nc.vector.tensor_tensor(out=tmpN[:], in0=h[:, s_, :], in1=h[:, s_, :], op=ALU.mult),
                         reads=[f'h{s_}'], writes=['tmpN'])
                    S.op('dve', lambda: nc.vector.reduce_sum(out=ss[:, 0:1], in_=tmpN[:], axis=AX.X), reads=['tmpN'], writes=['ss'])
                    S.op('dve', lambda: nc.vector.tensor_scalar(out=ss[:, 1:2], in0=ss[:, 0:1], scalar1=1.0 / D, scalar2=EPS, op0=ALU.mult, op1=ALU.add),
                         reads=['ss'], writes=['ss'])
                    S.op('act', lambda: nc.scalar.activation(out=ss[:, 2:3], in_=ss[:, 1:2], func=AF.Ln), reads=['ss'], writes=['ss'])
                    S.op('act', lambda: nc.scalar.activation(out=rs[:, 0:1], in_=ss[:, 2:3], func=AF.Exp, scale=-0.5), reads=['ss'], writes=['rs'])
                    S.op('dve', lambda: nc.vector.scalar_tensor_tensor(out=tmpN[:], in0=h[:, s_, :], scalar=rs[:, 0:1], in1=wfin[:], op0=ALU.mult, op1=ALU.mult),
                         reads=[f'h{s_}', 'rs', 'wfin'], writes=['tmpN'])
                    S.dma('sp', lambda: nc.sync.dma_start(out=out[otok0 + s_ * 128: otok0 + (s_ + 1) * 128, :], in_=tmpN[:]), reads=['tmpN'], writes=['out'], accum=True)
        S.wait_all('sp', ['out', 'dbgo'])
        for q in ('sp',):
            for k in range(S.NDMA):
                if S.dval[q][k] > 0:
                    nc.sync.wait_ge(S.dsem[q][k], S.dval[q][k])
        print(f"[build] instructions={S.ninst} waits={S.nwait}", flush=True)
    return nc


def core_inputs(cfg, inputs, slots):
    D = cfg['D']
    f = lambda a: np.ascontiguousarray(np.asarray(a, dtype=np.float32))
    npre = cfg.get('NPRE', 0) * TT

    def rows(a, seq, plo, phi, olo, ohi):
        a = np.asarray(a)
        pre = a[seq, plo:phi] if phi > plo else np.zeros((npre,) + a.shape[2:], np.float32)
        return np.concatenate([pre, a[seq, olo:ohi]], 0) if npre else a[seq, olo:ohi]

    x = np.concatenate([rows(inputs['x'], *sl) for sl in slots], 0)
    p = np.concatenate([np.concatenate([rows(inputs['p'][l], *sl) for sl in slots], 0) for l in range(2)], 0)
    NE = cfg['NE']
    d = {
        "x": f(x), "p": f(p),
        "w_in": f(inputs['w_in']).reshape(2 * D, -1),
        "w_out": f(inputs['w_out']).reshape(-1, D),
        "dwg": f(inputs['dense_w_gate'][0]), "dwu": f(inputs['dense_w_up'][0]), "dwd": f(inputs['dense_w_down'][0]),
        "mwr": f(inputs['moe_router'][0]),
        "mwg": f(inputs['moe_w_gate'][0]).reshape(NE * D, -1), "mwu": f(inputs['moe_w_up'][0]).reshape(NE * D, -1),
        "mwd": f(inputs['moe_w_down'][0]).reshape(-1, D),
        "pwg": f(inputs['ple_w_gate']).reshape(2 * D, D), "pwp": f(inputs['ple_w_proj']).reshape(-1, D),
        "nmix": f(inputs['norm_mix_w']), "nffn": f(inputs['norm_ffn_w']), "nple": f(inputs['norm_ple_w']),
        "nfin": f(inputs['final_norm_w']).reshape(1, D),
        "lbl": f(inputs['hgrn_lb_logits']), "hnw": f(inputs['hgrn_norm_w']), "gnw": f(inputs['gla_norm_w']),
        "gw2": f(inputs['gla_gate_w2']).reshape(-1, inputs['gla_gate_w2'].shape[-1]), "gb": f(inputs['gla_gate_b']),
        "cst": make_consts(),
    }
    return d


FULL_CFG = dict(D=2048, T=4096, NSEQ=1, HH=8, GH=4, F0=5632, FE=7168, NE=8)


def run(cfg, inputs, ncores, dbg=False, trace=False, split=1):
    B, Tfull = inputs['x'].shape[0], inputs['x'].shape[1]
    D = cfg['D']
    if split == 1:
        nseq = B // ncores
        cfg = dict(cfg, NSEQ=nseq, T=Tfull, NPRE=0)
        slots = [[(c * nseq + i, 0, 0, 0, Tfull) for i in range(nseq)] for c in range(ncores)]
    else:
        assert ncores == 2 * B
        hl = Tfull // 2
        cfg = dict(cfg, NSEQ=1, T=Tfull, NPRE=hl // TT)
        slots = [[(c // 2, 0, hl * (c % 2), hl * (c % 2), hl * (c % 2) + hl)] for c in range(ncores)]
    nc = build_program(cfg, dbg=dbg)
    in_maps = [core_inputs(cfg, inputs, slots[c]) for c in range(ncores)]
    res = run_bass_kernel_spmd(nc, in_maps, core_ids=list(range(ncores)), trace=trace)
    o = np.zeros((B, Tfull, D), np.float32)
    for c in range(ncores):
        r = res.results[c]["out"]
        off = 0
        for (sq, plo, phi, olo, ohi) in slots[c]:
            o[sq, olo:ohi] = r[off:off + (ohi - olo)]
            off += ohi - olo
    if dbg:
        return o, [r["dbg"] for r in res.results], res
    return o


def kernel(**inputs):
    return run(FULL_CFG, inputs, ncores=8, split=2)
```

```python
import contextlib
import numpy as np
import concourse.bass as bass
import concourse.mybir as mybir
from concourse.bass_utils import run_bass_kernel_spmd

F32 = mybir.dt.float32
BF16 = mybir.dt.bfloat16
AF = mybir.ActivationFunctionType
ALU = mybir.AluOpType
AX = mybir.AxisListType
EPS = 1e-6
TT = 512
NST = TT // 128


class Sched:
    NDMA = 8

    def __init__(self, nc, stack):
        self.nc = nc
        self.E = {'pe': nc.tensor, 'act': nc.scalar, 'dve': nc.vector, 'pool': nc.gpsimd, 'sp': nc.sync}
        self.sem = {k: stack.enter_context(nc.semaphore("s_" + k)) for k in self.E}
        self.cnt = {k: 0 for k in self.E}
        self.dsem = {q: [stack.enter_context(nc.semaphore(f"d_{q}{i}")) for i in range(self.NDMA)]
                     for q in ('pool', 'sp')}
        self.dval = {q: [0] * self.NDMA for q in self.dsem}
        self.dn = {q: 0 for q in self.dsem}
        self.known = {k: {} for k in self.E}
        self.semobj = {}
        self.lastw = {}
        self.readers = {}
        self.ninst = 0
        self.nwait = 0

    def _tok(self, sem, val):
        self.semobj[id(sem)] = sem
        return (id(sem), val)

    def _need(self, e, toks):
        kn = self.known[e]
        best = {}
        for t in toks:
            if t is None:
                continue
            sid, v = t
            if kn.get(sid, 0) >= v:
                continue
            if best.get(sid, 0) < v:
                best[sid] = v
        for sid, v in best.items():
            self.E[e].wait_ge(self.semobj[sid], v)
            kn[sid] = v
            self.nwait += 1

    def _deps(self, reads, writes):
        toks = []
        for r in reads:
            toks.extend(self.lastw.get(r, ()))
        for w in writes:
            toks.extend(self.lastw.get(w, ()))
            for sid, v in self.readers.get(w, {}).items():
                toks.append((sid, v))
        return toks

    def _commit(self, tok, reads, writes, accum=False):
        for r in reads:
            d = self.readers.setdefault(r, {})
            if d.get(tok[0], 0) < tok[1]:
                d[tok[0]] = tok[1]
        for w in writes:
            if accum:
                self.lastw[w] = tuple(self.lastw.get(w, ())) + (tok,)
            else:
                self.lastw[w] = (tok,)
                self.readers[w] = {}

    def op(self, e, fn, reads=(), writes=()):
        psr = [r for r in reads if r.startswith('ps') and r not in writes]
        if psr:
            writes = list(writes) + psr
        toks = self._deps(reads, writes)
        if e == 'pe':
            own = id(self.sem['pe'])
            toks = [t for t in toks if t is not None and t[0] != own]
        self._need(e, toks)
        ins = fn()
        self.cnt[e] += 1
        ins.then_inc(self.sem[e], 1)
        self._commit(self._tok(self.sem[e], self.cnt[e]), reads, writes)
        self.ninst += 1

    def burst(self, fns, reads=(), writes=()):
        psr = [r for r in reads if r.startswith('ps') and r not in writes]
        if psr:
            writes = list(writes) + psr
        own = id(self.sem['pe'])
        toks = [t for t in self._deps(reads, writes) if t is not None and t[0] != own]
        self._need('pe', toks)
        for fn in fns[:-1]:
            fn()
            self.ninst += 1
        ins = fns[-1]()
        self.cnt['pe'] += 1
        ins.then_inc(self.sem['pe'], 1)
        self._commit(self._tok(self.sem['pe'], self.cnt['pe']), reads, writes)
        self.ninst += 1

    def dma(self, q, fn, reads=(), writes=(), accum=False):
        k = self.dn[q] % self.NDMA
        self.dn[q] += 1
        toks = self._deps(reads, () if accum else writes)
        if self.dval[q][k] > 0:
            toks.append(self._tok(self.dsem[q][k], self.dval[q][k]))
        self._need(q, toks)
        ins = fn()
        self.dval[q][k] += 16
        ins.then_inc(self.dsem[q][k], 16)
        self._commit(self._tok(self.dsem[q][k], self.dval[q][k]), reads, writes, accum=accum)
        self.ninst += 1

    def alias_barrier(self, src, dst):
        toks = []
        for k in src:
            toks.extend(self.lastw.get(k, ()))
            toks.extend(self.readers.get(k, {}).items())
        for d in dst:
            self.lastw[d] = tuple(self.lastw.get(d, ())) + tuple(toks)

    def wait_all(self, e, keys):
        toks = []
        for k in keys:
            toks.extend(self.lastw.get(k, ()))
        self._need(e, toks)


def make_consts():
    t = np.arange(128)
    same = (t[:, None] // 64) == (t[None, :] // 64)
    ident = np.eye(128, dtype=np.float32)
    mask = (same & (t[:, None] <= t[None, :])).astype(np.float32)
    mtri = mask.copy()
    mlast = np.stack([(t // 64 == 0), (t // 64 == 1)], 1).astype(np.float32)
    mcat = np.concatenate([mtri, mlast], 1)
    mdec = (same & (t[:, None] > t[None, :])).astype(np.float32)
    ustrict = (t[:, None] < t[None, :]).astype(np.float32)
    iota = np.tile(np.arange(256, dtype=np.float32)[None, :], (128, 1))
    return np.concatenate([ident, mask, mcat, mdec, mcat / 16.0, mdec / 16.0, ustrict, iota], 1).astype(np.float32)


C_ID, C_MASK, C_MCAT, C_MDEC, C_MCATG, C_MDECG = 0, 128, 256, 386, 514, 644
C_US, C_IOTA = 772, 900
C_TOT = 1156


def build_program(cfg, dbg=False):
    D, T, NSEQ = cfg['D'], cfg['T'], cfg['NSEQ']
    HH, GH, F0, FE, NE = cfg['HH'], cfg['GH'], cfg['F0'], cfg['FE'], cfg['NE']
    PL, R = 256, 16
    KT = D // 128
    HW, GK, GW = HH * 128, GH * 128, GH * 256
    MIXW = HW + GW
    MKT = MIXW // 128
    INC = 4 * HW + 2 * GK + GW + R + GW
    NPRE = cfg.get('NPRE', 0)
    NTILE = T // TT
    NOWN = NTILE - NPRE
    NTOK = NSEQ * T
    NTOKO = NSEQ * NOWN * TT
    assert T % TT == 0 and D % 512 == 0 and F0 % 512 == 0 and FE % 512 == 0
    o_hq, o_hf, o_hi, o_hg = 0, HW, 2 * HW, 3 * HW
    o_gq = 4 * HW
    o_gk = o_gq + GK
    o_gv = o_gk + GK
    o_glr = o_gv + GW
    o_gg = o_glr + R
    HALF = 24
    SLOTC = max(KT, 16) * 512

    nc = bass.Bass("TRN2", target_bir_lowering=False)

    def din(name, shape):
        return nc.dram_tensor(name, list(shape), F32, kind="ExternalInput").ap()

    x = din("x", [NTOK, D])
    p = din("p", [2 * NTOK, PL])
    w_in = din("w_in", [2 * D, INC])
    w_out = din("w_out", [2 * MIXW, D])
    dwg = din("dwg", [D, F0]); dwu = din("dwu", [D, F0]); dwd = din("dwd", [F0, D])
    mwr = din("mwr", [D, NE])
    mwg = din("mwg", [NE * D, FE]); mwu = din("mwu", [NE * D, FE]); mwd = din("mwd", [NE * FE, D])
    pwg = din("pwg", [2 * D, D]); pwp = din("pwp", [2 * PL, D])
    nmix = din("nmix", [2, D]); nffn = din("nffn", [2, D]); nple = din("nple", [2, D]); nfin = din("nfin", [1, D])
    lbl = din("lbl", [2, HW]); hnw = din("hnw", [2, HW]); gnw = din("gnw", [2, GW])
    gw2 = din("gw2", [2 * R, GK]); gb = din("gb", [2, GK])
    cst = din("cst", [128, C_TOT])
    out = nc.dram_tensor("out", [NTOKO, D], F32, kind="ExternalOutput").ap()
    if dbg:
        dbgo = nc.dram_tensor("dbg", [8 * NTOK, D], F32, kind="ExternalOutput").ap()

    with contextlib.ExitStack() as st:
        S = Sched(nc, st)

        def sb(name, shape, dt=F32):
            return st.enter_context(nc.sbuf_tensor(name, list(shape), dt))

        h = sb("h", [128, NST, D])
        hnT = sb("hnT", [128, KT, TT], BF16)
        tmpN = sb("tmpN", [128, D])
        wsl = [sb(f"wsl{i}", [128, SLOTC], BF16) for i in range(3)]
        actT = sb("actT", [128, max(HALF, MKT), TT], BF16)
        oT = actT
        ptile = sb("ptile", [128, NST, PL])
        pT = sb("pT", [128, PL // 128, TT], BF16)
        wfin = sb("wfin", [128, D])
        cs = sb("cs", [128, C_TOT])
        NCB, HNB, GNB = 0, 6 * KT, 6 * KT + 2 * HH
        assert GNB + 4 * GH <= 128
        allcol = sb("allcol", [128, 128]); lbcol = sb("lbcol", [128, 128])
        lbrow = sb("lbrow", [128, HW]); omlrow = sb("omlrow", [128, HW]); lbtmp = tmpN[:, 0:HW]
        omlc = sb("omlc", [128, HH])
        w2aug = sb("w2aug", [32, 2, GK], BF16)
        glrT = sb("glrT", [32, TT], BF16)
        onesh = sb("onesh", [128, 128], BF16); onesg = sb("onesg", [128, 128], BF16)
        Sh = sb("Sh", [128, 2, HH, 128]); Sg = sb("Sg", [128, 2, GH, 256])
        Sb = [sb(f"Sb{i}", [128, 256], BF16) for i in range(2)]
        qT = sb("qT", [128, TT]); kT = sb("kT", [128, TT]); gateT = sb("gateT", [128, 2, TT])
        loga = sb("loga", [128, NST, 128]); ktok = sb("ktok", [128, NST, 128])
        kdec = sb("kdec", [128, NST, 128], BF16); V = sb("V", [128, NST, 256], BF16)
        qh = sb("qh", [128, NST, 128], BF16); kh = sb("kh", [128, NST, 128], BF16)
        E1 = [sb(f"E1{i}", [128, 130]) for i in range(2)]
        E2 = [sb(f"E2{i}", [128, 128]) for i in range(2)]
        D1 = [sb(f"D1{i}", [128, 128]) for i in range(2)]
        AT = [sb(f"AT{i}", [128, 128], BF16) for i in range(2)]
        osq = [sb(f"osq{i}", [128, 2, 128], BF16) for i in range(2)]
        rstdT = [sb(f"rstdT{i}", [128, 128]) for i in range(2)]
        otmp = [sb(f"otmp{i}", [128, 128]) for i in range(2)]
        stmp = [sb(f"stmp{i}", [128, 128]) for i in range(2)]
        ss = sb("ss", [128, 8]); rs = sb("rs", [128, 8])
        gtmp = [sb(f"gtmp{i}", [128, 512]) for i in range(2)]
        lg = sb("lg", [128, NST, NE]); lg2 = sb("lg2", [128, NE]); eq1 = sb("eq1", [128, NE]); eq2 = sb("eq2", [128, NE])
        gwt = sb("gwt", [128, NST, NE]); m12 = sb("m12", [128, 4])
        CAP = cfg.get('CAP', 256); NSL = CAP // 128
        maskf = sb("maskf", [128, NST, NE]); maskb = sb("maskb", [128, NST, NE], BF16); posm = sb("posm", [128, NST, NE])
        usb = sb("usb", [128, 128], BF16); ones1b = sb("ones1b", [128, 128], BF16); identb = sb("identb", [128, 128], BF16)
        XT = hnT[:, :, 0:256]
        hnraw = actT[:, 8:24, :].rearrange("p a b -> p (a b)")[:, 0:NST * D].rearrange("p (s d) -> p s d", d=D)
        actT2 = actT[:, 0:8, :].rearrange("p a b -> p (a b)").rearrange("p (f c) -> p f c", c=256)
        assert KT >= 16 or D * NST <= 16 * 512
        ps = [st.enter_context(nc.psum_tensor(f"ps{i}", [128, 512], F32)) for i in range(7)]
        psb = st.enter_context(nc.psum_tensor("psb", [128, 1024], BF16))
        psn = [0]

        def P():
            i = psn[0] % 7
            psn[0] += 1
            return ps[i], f"ps{i}"

        ident = cs[:, C_ID:C_ID + 128]
        maskBD = cs[:, C_MASK:C_MASK + 128]

        S.dma('sp', lambda: nc.sync.dma_start(out=cs[:], in_=cst), writes=['cs'])
        S.dma('sp', lambda: nc.sync.dma_start(out=wfin[:], in_=nfin[0:1, :].partition_broadcast(128)), writes=['wfin'])
        S.op('dve', lambda: nc.vector.memset(tmpN[:, 0:256], 0.0), writes=['tmpN'])
        stg1, stg2 = tmpN[:, 0:128], tmpN[:, 128:256]
        for i, src in enumerate([nmix, nffn, nple]):
            S.dma('sp', lambda: nc.sync.dma_start(out=tmpN[2 * KT * i:2 * KT * (i + 1), 0:128], in_=src.rearrange("l (k p) -> (l k) p", p=128)),
                  writes=['tmpN'], accum=(i > 0))
        S.dma('sp', lambda: nc.sync.dma_start(out=tmpN[HNB:HNB + 2 * HH, 0:128], in_=hnw.rearrange("l (k p) -> (l k) p", p=128)), writes=['tmpN'], accum=True)
        S.dma('sp', lambda: nc.sync.dma_start(out=tmpN[GNB:GNB + 4 * GH, 0:128], in_=gnw.rearrange("l (k p) -> (l k) p", p=128)), writes=['tmpN'], accum=True)
        S.dma('sp', lambda: nc.sync.dma_start(out=tmpN[0:2 * HH, 128:256], in_=lbl.rearrange("l (k p) -> (l k) p", p=128)), writes=['tmpN'], accum=True)
        for (src_, dst_, nm) in ((stg1, allcol, 'allcol'), (stg2, lbcol, 'lbcol')):
            pt, pk = P()
            S.op('pe', lambda: nc.tensor.transpose(out=pt[:, 0:128], in_=src_, identity=ident), reads=['tmpN', 'cs'], writes=[pk])
            S.op('dve', lambda: nc.vector.tensor_copy(out=dst_[:], in_=pt[:, 0:128]), reads=[pk], writes=[nm])
        for l in range(2):
            S.dma('pool', lambda: nc.gpsimd.dma_start(out=w2aug[0:R, l, :], in_=gw2[l * R:(l + 1) * R, :]), writes=['w2aug'], accum=True)
            S.dma('pool', lambda: nc.gpsimd.dma_start(out=w2aug[R:R + 1, l, :], in_=gb[l:l + 1, :]), writes=['w2aug'], accum=True)
        S.dma('sp', lambda: nc.sync.dma_start(out=lbrow[:], in_=lbl[1:2, :].partition_broadcast(128)), writes=['lbrow'])
        S.dma('sp', lambda: nc.sync.dma_start(out=lbtmp, in_=lbl[0:1, :].partition_broadcast(128)), writes=['tmpN'])
        S.op('dve', lambda: nc.vector.tensor_tensor(out=lbtmp, in0=lbrow[:], in1=lbtmp, op=ALU.subtract),
             reads=['lbrow', 'tmpN'], writes=['tmpN'])
        S.op('act', lambda: nc.scalar.activation(out=lbrow[:], in_=lbtmp, func=AF.Sigmoid), reads=['tmpN'], writes=['lbrow'])
        S.op('dve', lambda: nc.vector.tensor_scalar(out=omlrow[:], in0=lbrow[:], scalar1=-1.0, scalar2=1.0, op0=ALU.mult, op1=ALU.add),
             reads=['lbrow'], writes=['omlrow'])
        S.op('dve', lambda: nc.vector.tensor_tensor(out=omlc[:], in0=lbcol[:, 0:HH], in1=lbcol[:, HH:2 * HH], op=ALU.subtract),
             reads=['lbcol'], writes=['omlc'])
        S.op('act', lambda: nc.scalar.activation(out=omlc[:], in_=omlc[:], func=AF.Sigmoid), reads=['omlc'], writes=['omlc'])
        S.op('dve', lambda: nc.vector.memset(onesh[:], 1.0 / 128.0), writes=['onesh'])
        S.op('dve', lambda: nc.vector.memset(onesg[:], 1.0 / 256.0), writes=['onesg'])
        S.op('dve', lambda: nc.vector.memset(glrT[:], 1.0), writes=['glrT'])
        S.op('dve', lambda: nc.vector.memset(ones1b[:], 1.0), writes=['ones1b'])
        S.op('dve', lambda: nc.vector.tensor_copy(out=usb[:], in_=cs[:, C_US:C_US + 128]), reads=['cs'], writes=['usb'])
        S.op('dve', lambda: nc.vector.tensor_copy(out=identb[:], in_=cs[:, C_ID:C_ID + 128]), reads=['cs'], writes=['identb'])

        wn = [0]

        def wload(parts, ksplit=4):
            i = wn[0] % 3
            wn[0] += 1
            tile_, key = wsl[i], f"wsl{i}"
            first = [True]
            for (src, nk, coff, ncols, stride) in parts:
                dstv = tile_[:, 0:nk * stride].rearrange("p (k c) -> p k c", c=stride)
                srcv = src.rearrange("(k p) c -> p k c", p=128)
                for k0 in range(0, nk, ksplit):
                    k1 = min(nk, k0 + ksplit)
                    for c0 in range(0, ncols, 512):
                        c1 = min(ncols, c0 + 512)
                        S.dma('pool', lambda: nc.gpsimd.dma_start(out=dstv[:, k0:k1, coff + c0:coff + c1], in_=srcv[:, k0:k1, c0:c1]),
                              writes=[key], accum=not first[0])
                        first[0] = False
            return tile_, key

        def wview(tile_, nk, stride):
            return tile_[:, 0:nk * stride].rearrange("p (k c) -> p k c", c=stride)

        def ncix(norm_idx, k):
            return NCB + (norm_idx // 2) * 2 * KT + (norm_idx % 2) * KT + k

        def rmsnorm_T(norm_idx, keep_tok=False):
            for s_ in range(NST):
                S.op('dve', lambda: nc.vector.tensor_tensor(out=tmpN[:], in0=h[:, s_, :], in1=h[:, s_, :], op=ALU.mult),
                     reads=[f'h{s_}'], writes=['tmpN'])
                S.op('dve', lambda: nc.vector.reduce_sum(out=ss[:, 0:1], in_=tmpN[:], axis=AX.X), reads=['tmpN'], writes=['ss'])
                S.op('dve', lambda: nc.vector.tensor_scalar(out=ss[:, 1:2], in0=ss[:, 0:1], scalar1=1.0 / D, scalar2=EPS,
                                                            op0=ALU.mult, op1=ALU.add), reads=['ss'], writes=['ss'])
                S.op('act', lambda: nc.scalar.activation(out=ss[:, 2:3], in_=ss[:, 1:2], func=AF.Sqrt), reads=['ss'], writes=['ss'])
                S.op('dve', lambda: nc.vector.reciprocal(out=rs[:, 0:1], in_=ss[:, 2:3]), reads=['ss'], writes=['rs'])
                S.op('act', lambda: nc.scalar.mul(out=tmpN[:], in_=h[:, s_, :], mul=rs[:, 0:1]),
                     reads=[f'h{s_}', 'rs'], writes=['tmpN'])
                if keep_tok:
                    S.op('dve', lambda: nc.vector.tensor_copy(out=hnraw[:, s_, :], in_=tmpN[:]), reads=['tmpN'], writes=['hnraw'])
                for k0 in range(0, KT, 4):
                    pt, pk = P()
                    for k in range(k0, min(k0 + 4, KT)):
                        S.op('pe', lambda: nc.tensor.transpose(out=pt[:, (k - k0) * 128:(k - k0 + 1) * 128],
                                                               in_=tmpN[:, k * 128:(k + 1) * 128], identity=ident),
                             reads=['tmpN', 'cs'], writes=[pk])
                    for k in range(k0, min(k0 + 4, KT)):
                        e = 'act' if ((k0 // 4) % 2) else 'dve'
                        if e == 'act':
                            S.op('act', lambda: nc.scalar.mul(out=hnT[:, k, s_ * 128:(s_ + 1) * 128],
                                                              in_=pt[:, (k - k0) * 128:(k - k0 + 1) * 128],
                                                              mul=allcol[:, ncix(norm_idx, k):ncix(norm_idx, k) + 1]),
                                 reads=[pk, 'allcol'], writes=['hnT'])
                        else:
                            S.op('dve', lambda: nc.vector.tensor_scalar(out=hnT[:, k, s_ * 128:(s_ + 1) * 128],
                                                                        in0=pt[:, (k - k0) * 128:(k - k0 + 1) * 128],
                                                                        scalar1=allcol[:, ncix(norm_idx, k):ncix(norm_idx, k) + 1], scalar2=None,
                                                                        op0=ALU.mult),
                                 reads=[pk, 'allcol'], writes=['hnT'])

        def proj_fm(wv, wkey, c0, m, pt, pk, ncols=TT):
            S.burst([(lambda k=k: nc.tensor.matmul(pt[0:m, 0:ncols], lhsT=wv[:, k, c0:c0 + m], rhs=hnT[:, k, 0:ncols],
                                                   start=(k == 0), stop=(k == KT - 1))) for k in range(KT)],
                    reads=[wkey, 'hnT'], writes=[pk])

        def proj_tm(wv, wkey, c0, n, s_, pt, pk):
            S.burst([(lambda k=k: nc.tensor.matmul(pt[:, 0:n], lhsT=hnT[:, k, s_ * 128:(s_ + 1) * 128], rhs=wv[:, k, c0:c0 + n],
                                                   start=(k == 0), stop=(k == KT - 1))) for k in range(KT)],
                    reads=[wkey, 'hnT'], writes=[pk])

        def chunk_attn(l, dv, Sst, skey, mc0, md0, normcol, ones_t, okt0, state_only=False):
            nb = dv // 128
            for s_ in range(NST):
                b = s_ % 2
                tsl = slice(s_ * 128, (s_ + 1) * 128)
                g1, g1k = P()
                S.op('pe', lambda: nc.tensor.matmul(g1[:, 0:130], lhsT=loga[:, s_, :], rhs=cs[:, mc0:mc0 + 130], start=True, stop=True),
                     reads=['loga', 'cs'], writes=[g1k])
                g2, g2k = P()
                S.op('pe', lambda: nc.tensor.matmul(g2[:, 0:128], lhsT=cs[:, md0:md0 + 128], rhs=loga[:, s_, :], start=True, stop=True),
                     reads=['loga', 'cs'], writes=[g2k])
                S.op('act', lambda: nc.scalar.activation(out=E1[b][:], in_=g1[:, 0:130], func=AF.Exp), reads=[g1k], writes=[f'E1{b}'])
                S.op('act', lambda: nc.scalar.activation(out=E2[b][:], in_=g1[:, 0:128], func=AF.Exp, scale=-1.0), reads=[g1k], writes=[f'E2{b}'])
                S.op('act', lambda: nc.scalar.activation(out=D1[b][:], in_=g2[:, 0:128], func=AF.Exp), reads=[g2k], writes=[f'D1{b}'])
                S.op('dve', lambda: nc.vector.tensor_tensor(out=kdec[:, s_, :], in0=ktok[:, s_, :], in1=D1[b][:], op=ALU.mult),
                     reads=['ktok', f'D1{b}'], writes=[f'kdec{s_}'])
                if state_only:
                    for c in range(2):
                        u_, uk = P()
                        S.op('pe', lambda: nc.tensor.matmul(u_[:, 0:dv], lhsT=kdec[c * 64:(c + 1) * 64, s_, :], rhs=V[c * 64:(c + 1) * 64, s_, 0:dv],
                                                            start=True, stop=True),
                             reads=[f'kdec{s_}', 'V'], writes=[uk])
                        S.op('dve', lambda: nc.vector.scalar_tensor_tensor(out=Sst, in0=Sst, scalar=E1[b][:, 128 + c:129 + c], in1=u_[:, 0:dv],
                                                                           op0=ALU.mult, op1=ALU.add),
                             reads=[skey, f'E1{b}', uk], writes=[skey])
                    continue
                S.op('dve', lambda: nc.vector.tensor_tensor(out=qh[:, s_, :], in0=qT[:, tsl], in1=E1[b][:, 0:128], op=ALU.mult),
                     reads=['qT', f'E1{b}'], writes=[f'qh{s_}'])
                S.op('dve', lambda: nc.vector.tensor_tensor(out=kh[:, s_, :], in0=kT[:, tsl], in1=E2[b][:], op=ALU.mult),
                     reads=['kT', f'E2{b}'], writes=[f'kh{s_}'])
                sc, sck = P()
                S.op('pe', lambda: nc.tensor.matmul(sc[:, 0:128], lhsT=kh[:, s_, :], rhs=qh[:, s_, :], start=True, stop=True),
                     reads=[f'kh{s_}', f'qh{s_}'], writes=[sck])
                S.op('dve', lambda: nc.vector.tensor_tensor(out=AT[b][:], in0=sc[:, 0:128], in1=maskBD, op=ALU.mult),
                     reads=[sck, 'cs'], writes=[f'AT{b}'])
                obl = [P() for _ in range(nb)]
                for blk in range(nb):
                    S.op('pe', lambda: nc.tensor.matmul(obl[blk][0][:, 0:128], lhsT=V[:, s_, blk * 128:(blk + 1) * 128], rhs=AT[b][:], start=True, stop=False),
                         reads=['V', f'AT{b}'], writes=[obl[blk][1]])
                for c in range(2):
                    sbt, sbk = Sb[c], f"Sb{c}"
                    S.op('act', lambda: nc.scalar.copy(out=sbt[:, 0:dv], in_=Sst), reads=[skey], writes=[sbk])
                    for blk in range(nb):
                        S.op('pe', lambda: nc.tensor.matmul(obl[blk][0][:, c * 64:(c + 1) * 64],
                                                            lhsT=sbt[:, blk * 128:(blk + 1) * 128], rhs=qh[:, s_, c * 64:(c + 1) * 64],
                                                            start=False, stop=(c == 1)),
                             reads=[sbk, f'qh{s_}'], writes=[obl[blk][1]])
                    u_, uk = P()
                    S.op('pe', lambda: nc.tensor.matmul(u_[:, 0:dv], lhsT=kdec[c * 64:(c + 1) * 64, s_, :], rhs=V[c * 64:(c + 1) * 64, s_, 0:dv],
                                                        start=True, stop=True),
                         reads=[f'kdec{s_}', 'V'], writes=[uk])
                    S.op('dve', lambda: nc.vector.scalar_tensor_tensor(out=Sst, in0=Sst, scalar=E1[b][:, 128 + c:129 + c], in1=u_[:, 0:dv],
                                                                       op0=ALU.mult, op1=ALU.add),
                         reads=[skey, f'E1{b}', uk], writes=[skey])
                for blk in range(nb):
                    S.op('act', lambda: nc.scalar.activation(out=osq[b][:, blk, :], in_=obl[blk][0][:, 0:128], func=AF.Square),
                         reads=[obl[blk][1]], writes=[f'osq{b}'])
                n_, nk_ = P()
                for blk in range(nb):
                    S.op('pe', lambda: nc.tensor.matmul(n_[:, 0:128], lhsT=ones_t[:], rhs=osq[b][:, blk, :], start=(blk == 0), stop=(blk == nb - 1)),
                         reads=[f'osq{b}', 'ones'], writes=[nk_])
                S.op('dve', lambda: nc.vector.tensor_scalar(out=rstdT[b][:], in0=n_[:, 0:128], scalar1=EPS, scalar2=None, op0=ALU.add),
                     reads=[nk_], writes=[f'rstdT{b}'])
                S.op('act', lambda: nc.scalar.activation(out=rstdT[b][:], in_=rstdT[b][:], func=AF.Sqrt), reads=[f'rstdT{b}'], writes=[f'rstdT{b}'])
                S.op('dve', lambda: nc.vector.reciprocal(out=rstdT[b][:], in_=rstdT[b][:]), reads=[f'rstdT{b}'], writes=[f'rstdT{b}'])
                for blk in range(nb):
                    S.op('dve', lambda: nc.vector.scalar_tensor_tensor(out=otmp[b][:], in0=obl[blk][0][:, 0:128],
                                                                       scalar=normcol[:, blk:blk + 1], in1=rstdT[b][:], op0=ALU.mult, op1=ALU.mult),
                         reads=[obl[blk][1], f'rstdT{b}', 'allcol'], writes=[f'otmp{b}'])
                    S.op('dve', lambda: nc.vector.tensor_tensor(out=oT[:, okt0 + blk, tsl], in0=otmp[b][:], in1=gateT[:, blk, tsl], op=ALU.mult),
                         reads=[f'otmp{b}', 'gateT'], writes=['oT'])

        MS = cfg.get('mixsub', 99)

        def mixer(l, state_only=False):
            so = state_only
            S.alias_barrier(['actT', 'hnraw'], ['oT'])
            rmsnorm_T(0 + l)
            for hd in range(HH):
                wt, wk = wload([(w_in[l * D:(l + 1) * D, o + hd * 128: o + (hd + 1) * 128], KT, j * 128, 128, 512)
                                for j, o in enumerate([o_hq, o_hf, o_hi, o_hg]) if (not so) or j in (1, 2)])
                wv = wview(wt, KT, 512)
                if not so:
                    pa, pak = P()
                    proj_fm(wv, wk, 0, 128, pa, pak)
                    S.op('act', lambda: nc.scalar.activation(out=qT[:], in_=pa[:, :], func=AF.Silu), reads=[pak], writes=['qT'])
                    pb, pbk = P()
                    proj_fm(wv, wk, 128, 128, pb, pbk)
                    S.op('act', lambda: nc.scalar.activation(out=kT[:], in_=pb[:, :], func=AF.Sigmoid, scale=-1.0), reads=[pbk], writes=['kT'])
                    if l == 1:
                        S.op('dve', lambda: nc.vector.tensor_scalar(out=kT[:], in0=kT[:], scalar1=omlc[:, hd:hd + 1], scalar2=None, op0=ALU.mult),
                             reads=['kT', 'omlc'], writes=['kT'])
                    pc, pck = P()
                    proj_fm(wv, wk, 384, 128, pc, pck)
                    S.op('act', lambda: nc.scalar.activation(out=gateT[:, 0, :], in_=pc[:, :], func=AF.Sigmoid), reads=[pck], writes=['gateT'])
                for s_ in range(NST):
                    b = s_ % 2
                    pd, pdk = P()
                    proj_tm(wv, wk, 128, 256, s_, pd, pdk)
                    S.op('act', lambda: nc.scalar.activation(out=stmp[b][:], in_=pd[:, 0:128], func=AF.Sigmoid), reads=[pdk], writes=[f'stmp{b}'])
                    if l == 1:
                        hs = slice(hd * 128, (hd + 1) * 128)
                        S.op('dve', lambda: nc.vector.tensor_tensor(out=stmp[b][:], in0=stmp[b][:], in1=omlrow[:, hs], op=ALU.mult),
                             reads=[f'stmp{b}', 'omlrow'], writes=[f'stmp{b}'])
                        S.op('dve', lambda: nc.vector.tensor_tensor(out=stmp[b][:], in0=stmp[b][:], in1=lbrow[:, hs], op=ALU.add),
                             reads=[f'stmp{b}', 'lbrow'], writes=[f'stmp{b}'])
                    S.op('act', lambda: nc.scalar.activation(out=loga[:, s_, :], in_=stmp[b][:], func=AF.Ln), reads=[f'stmp{b}'], writes=['loga'])
                    S.op('dve', lambda: nc.vector.tensor_scalar(out=ktok[:, s_, :], in0=stmp[b][:], scalar1=-1.0, scalar2=1.0, op0=ALU.mult, op1=ALU.add),
                         reads=[f'stmp{b}'], writes=['ktok'])
                    S.op('act', lambda: nc.scalar.copy(out=V[:, s_, 0:128], in_=pd[:, 128:256]), reads=[pdk], writes=['V'])
                if MS >= 2:
                  chunk_attn(l, 128, Sh[:, l, hd, :], f"Sh{l}_{hd}", C_MCAT, C_MDEC, allcol[:, HNB + l * HH + hd:HNB + l * HH + hd + 1], onesh, hd, state_only=so)
            if MS < 3:
                return
            wt, wk = wload([(w_in[l * D:(l + 1) * D, o_glr:o_glr + R], KT, 0, R, 512)])
            wv = wview(wt, KT, 512)
            pg, pgk = P()
            proj_fm(wv, wk, 0, R, pg, pgk)
            S.op('act', lambda: nc.scalar.copy(out=glrT[0:R, :], in_=pg[0:R, :]), reads=[pgk], writes=['glrT'])
            for gh in range(GH if MS >= 4 else 0):
                gparts = [(w_in[l * D:(l + 1) * D, o_gq + gh * 128: o_gq + (gh + 1) * 128], KT, 0, 128, 512),
                          (w_in[l * D:(l + 1) * D, o_gk + gh * 128: o_gk + (gh + 1) * 128], KT, 128, 128, 512),
                          (w_in[l * D:(l + 1) * D, o_gv + gh * 256: o_gv + (gh + 1) * 256], KT, 256, 256, 512)]
                wt, wk = wload(gparts[1:] if so else gparts)
                wv = wview(wt, KT, 512)
                if not so:
                    wt2, wk2 = wload([(w_in[l * D:(l + 1) * D, o_gg + gh * 256: o_gg + (gh + 1) * 256], KT, 0, 256, 512)])
                    wv2 = wview(wt2, KT, 512)
                    pa, pak = P()
                    proj_fm(wv, wk, 0, 128, pa, pak)
                    S.op('act', lambda: nc.scalar.mul(out=qT[:], in_=pa[:, :], mul=float(128 ** -0.5)), reads=[pak], writes=['qT'])
                    pb, pbk = P()
                    proj_fm(wv, wk, 128, 128, pb, pbk)
                    S.op('act', lambda: nc.scalar.copy(out=kT[:], in_=pb[:, :]), reads=[pbk], writes=['kT'])
                    for blk in range(2):
                        pc, pck = P()
                        proj_fm(wv2, wk2, blk * 128, 128, pc, pck)
                        S.op('act', lambda: nc.scalar.activation(out=gateT[:, blk, :], in_=pc[:, :], func=AF.Silu), reads=[pck], writes=['gateT'])
                for s_ in range(NST):
                    b = s_ % 2
                    pd, pdk = P()
                    proj_tm(wv, wk, 128, 384, s_, pd, pdk)
                    S.op('act', lambda: nc.scalar.copy(out=ktok[:, s_, :], in_=pd[:, 0:128]), reads=[pdk], writes=['ktok'])
                    S.op('act', lambda: nc.scalar.copy(out=V[:, s_, :], in_=pd[:, 128:384]), reads=[pdk], writes=['V'])
                    pz, pzk = P()
                    S.op('pe', lambda: nc.tensor.matmul(pz[:, 0:128], lhsT=glrT[0:R + 1, s_ * 128:(s_ + 1) * 128],
                                                        rhs=w2aug[0:R + 1, l, gh * 128:(gh + 1) * 128], start=True, stop=True),
                         reads=['glrT', 'w2aug'], writes=[pzk])
                    S.op('act', lambda: nc.scalar.activation(out=stmp[b][:], in_=pz[:, 0:128], func=AF.Sigmoid), reads=[pzk], writes=[f'stmp{b}'])
                    S.op('act', lambda: nc.scalar.activation(out=loga[:, s_, :], in_=stmp[b][:], func=AF.Ln), reads=[f'stmp{b}'], writes=['loga'])
                chunk_attn(l, 256, Sg[:, l, gh, :], f"Sg{l}_{gh}", C_MCATG, C_MDECG, allcol[:, GNB + l * 2 * GH + 2 * gh:GNB + l * 2 * GH + 2 * gh + 2], onesg, HH + 2 * gh, state_only=so)
            for dc in range(D // 512 if (MS >= 5 and not so) else 0):
                wt, wk = wload([(w_out[l * MIXW:(l + 1) * MIXW, dc * 512:(dc + 1) * 512], MKT, 0, 512, 512)])
                wv = wview(wt, MKT, 512)
                for s_ in range(NST):
                    po, pok = P()
                    S.burst([(lambda k=k: nc.tensor.matmul(po[:, :], lhsT=oT[:, k, s_ * 128:(s_ + 1) * 128], rhs=wv[:, k, :],
                                                           start=(k == 0), stop=(k == MKT - 1))) for k in range(MKT)],
                            reads=['oT', wk], writes=[pok])
                    S.op('dve', lambda: nc.vector.tensor_tensor(out=h[:, s_, dc * 512:(dc + 1) * 512], in0=h[:, s_, dc * 512:(dc + 1) * 512],
                                                                in1=po[:, :], op=ALU.add),
                         reads=[pok, f'h{s_}'], writes=[f'h{s_}'])
            S.alias_barrier(['oT'], ['actT', 'hnraw'])

        def swiglu(wg, wu, wd, F, gate_col=None):
            NG = F // 512
            groups = list(range(NG))
            halves = [groups[i:i + HALF // 4] for i in range(0, NG, HALF // 4)]
            for hv in halves:
                nft = len(hv) * 4
                for gi, g in enumerate(hv):
                    wtg, wkg = wload([(wg[:, g * 512:(g + 1) * 512], KT, 0, 512, 512)])
                    wtu, wku = wload([(wu[:, g * 512:(g + 1) * 512], KT, 0, 512, 512)])
                    wvg, wvu = wview(wtg, KT, 512), wview(wtu, KT, 512)
                    for j in range(4):
                        b = j % 2
                        pg_, pgk_ = P()
                        proj_fm(wvg, wkg, j * 128, 128, pg_, pgk_)
                        pu_, puk_ = P()
                        proj_fm(wvu, wku, j * 128, 128, pu_, puk_)
                        S.op('act', lambda: nc.scalar.activation(out=gtmp[b][:], in_=pg_[:, :], func=AF.Silu), reads=[pgk_], writes=[f'gtmp{b}'])
                        S.op('dve', lambda: nc.vector.tensor_tensor(out=actT[:, gi * 4 + j, :], in0=gtmp[b][:], in1=pu_[:, :], op=ALU.mult),
                             reads=[f'gtmp{b}', puk_], writes=['actT'])
                for dc in range(D // 512):
                    accs = [P() for _ in range(NST)]
                    for f0 in range(0, nft, 16):
                        nf = min(16, nft - f0)
                        r0 = (hv[0] * 4 + f0) * 128
                        wt, wk = wload([(wd[r0:r0 + nf * 128, dc * 512:(dc + 1) * 512], nf, 0, 512, 512)])
                        wv = wview(wt, nf, 512)
                        S.burst([(lambda j=j, s_=s_: nc.tensor.matmul(accs[s_][0][:, :], lhsT=actT[:, f0 + j, s_ * 128:(s_ + 1) * 128], rhs=wv[:, j, :],
                                                                      start=(f0 + j == 0), stop=(f0 + j == nft - 1)))
                                 for j in range(nf) for s_ in range(NST)],
                                reads=['actT', wk], writes=[a[1] for a in accs])
                    for s_ in range(NST):
                        hsl = h[:, s_, dc * 512:(dc + 1) * 512]
                        if gate_col is None:
                            S.op('dve', lambda: nc.vector.tensor_tensor(out=hsl, in0=hsl, in1=accs[s_][0][:, :], op=ALU.add),
                                 reads=[accs[s_][1], f'h{s_}'], writes=[f'h{s_}'])
                        else:
                            S.op('dve', lambda: nc.vector.scalar_tensor_tensor(out=hsl, in0=accs[s_][0][:, :], scalar=gwt[:, s_, gate_col:gate_col + 1],
                                                                               in1=hsl, op0=ALU.mult, op1=ALU.add),
                                 reads=[accs[s_][1], f'h{s_}', 'gwt'], writes=[f'h{s_}'])

        def moe_sparse():
            S.alias_barrier(['hnT'], ['XT', 'Pe', 'PTe', 'Ydc0', 'Ydc1'])
            for s_ in range(NST):
                S.op('dve', lambda: nc.vector.tensor_scalar(out=maskf[:, s_, :], in0=gwt[:, s_, :], scalar1=0.0, scalar2=None, op0=ALU.is_gt),
                     reads=['gwt'], writes=['maskf'])
                S.op('dve', lambda: nc.vector.tensor_copy(out=maskb[:, s_, :], in_=maskf[:, s_, :]), reads=['maskf'], writes=['maskb'])
            for s_ in range(NST):
                pp, ppk = P()
                for s2 in range(s_):
                    S.op('pe', lambda: nc.tensor.matmul(pp[:, 0:NE], lhsT=ones1b[:], rhs=maskb[:, s2, :], start=(s2 == 0), stop=False),
                         reads=['ones1b', 'maskb'], writes=[ppk])
                S.op('pe', lambda: nc.tensor.matmul(pp[:, 0:NE], lhsT=usb[:], rhs=maskb[:, s_, :], start=(s_ == 0), stop=True),
                     reads=['usb', 'maskb'], writes=[ppk])
                S.op('dve', lambda: nc.vector.scalar_tensor_tensor(out=posm[:, s_, :], in0=pp[:, 0:NE], scalar=1.0, in1=maskf[:, s_, :], op0=ALU.add, op1=ALU.mult),
                     reads=[ppk, 'maskf'], writes=['posm'])
                S.op('dve', lambda: nc.vector.tensor_scalar(out=posm[:, s_, :], in0=posm[:, s_, :], scalar1=-1.0, scalar2=None, op0=ALU.add),
                     reads=['posm'], writes=['posm'])
            NG = FE // 512
            GP = 3
            for e in range(NE):
                for s_ in range(NST):
                    S.op('dve', lambda: nc.vector.tensor_scalar(out=hnT[:, s_, 256:256 + CAP], in0=cs[:, C_IOTA:C_IOTA + CAP], scalar1=posm[:, s_, e:e + 1],
                                                                scalar2=None, op0=ALU.is_equal),
                         reads=['cs', 'posm'], writes=['Pe'])
                for s_ in range(NST):
                    for t2 in range(NSL):
                        r = t2 * NST + s_
                        S.op('pe', lambda: nc.tensor.transpose(out=psb[:, r * 128:(r + 1) * 128], in_=hnT[:, s_, 256 + t2 * 128:256 + (t2 + 1) * 128], identity=identb[:]),
                             reads=['Pe', 'identb'], writes=['psb'])
                for t2 in range(NSL):
                    for hf in range(2):
                        S.op('act', lambda: nc.scalar.copy(out=hnT[:, 4 + t2 * 2 + hf, 256:512], in_=psb[:, (t2 * NST + 2 * hf) * 128:(t2 * NST + 2 * hf + 2) * 128]),
                             reads=['psb'], writes=['PTe'])
                for k0 in range(0, KT, 2):
                    pt, pk = P()
                    S.burst([(lambda k=k, s_=s_: nc.tensor.matmul(pt[:, (k - k0) * 256:(k - k0) * 256 + CAP], lhsT=hnraw[:, s_, k * 128:(k + 1) * 128],
                                                                  rhs=hnT[:, s_, 256:256 + CAP], start=(s_ == 0), stop=(s_ == NST - 1)))
                             for k in range(k0, min(k0 + 2, KT)) for s_ in range(NST)],
                            reads=['hnraw', 'Pe'], writes=[pk])
                    for k in range(k0, min(k0 + 2, KT)):
                        eng = 'act' if ((k0 // 2) % 2) else 'dve'
                        src_ = pt[:, (k - k0) * 256:(k - k0) * 256 + CAP]
                        col = allcol[:, ncix(3, k):ncix(3, k) + 1]
                        if eng == 'act':
                            S.op('act', lambda: nc.scalar.mul(out=XT[:, k, 0:CAP], in_=src_, mul=col), reads=[pk, 'allcol'], writes=['XT'])
                        else:
                            S.op('dve', lambda: nc.vector.tensor_scalar(out=XT[:, k, 0:CAP], in0=src_, scalar1=col, scalar2=None, op0=ALU.mult),
                                 reads=[pk, 'allcol'], writes=['XT'])
                wg_e, wu_e, wd_e = mwg[e * D:(e + 1) * D, :], mwu[e * D:(e + 1) * D, :], mwd[e * FE:(e + 1) * FE, :]
                for g0 in range(0, NG, GP):
                    gs = list(range(g0, min(NG, g0 + GP)))
                    nft = len(gs) * 4
                    for gi, g in enumerate(gs):
                      for hh in range(2):
                        cc = g * 512 + hh * 256
                        wtg, wkg = wload([(wg_e[:, cc:cc + 256], KT, 0, 256, 512), (wu_e[:, cc:cc + 256], KT, 256, 256, 512)], ksplit=8)
                        wvg = wview(wtg, KT, 512)
                        for jj in range(2):
                            j = hh * 2 + jj
                            b = j % 2
                            pg_, pgk_ = P()
                            pu_, puk_ = P()
                            S.burst([(lambda k=k: nc.tensor.matmul(pg_[:, 0:CAP], lhsT=wvg[:, k, jj * 128:(jj + 1) * 128], rhs=XT[:, k, 0:CAP],
                                                                   start=(k == 0), stop=(k == KT - 1))) for k in range(KT)], reads=[wkg, 'XT'], writes=[pgk_])
                            S.burst([(lambda k=k: nc.tensor.matmul(pu_[:, 0:CAP], lhsT=wvg[:, k, 256 + jj * 128:256 + (jj + 1) * 128], rhs=XT[:, k, 0:CAP],
                                                                   start=(k == 0), stop=(k == KT - 1))) for k in range(KT)], reads=[wkg, 'XT'], writes=[puk_])
                            S.op('act', lambda: nc.scalar.activation(out=gtmp[b][:, 0:CAP], in_=pg_[:, 0:CAP], func=AF.Silu), reads=[pgk_], writes=[f'gtmp{b}'])
                            S.op('dve', lambda: nc.vector.tensor_tensor(out=actT2[:, gi * 4 + j, 0:CAP], in0=gtmp[b][:, 0:CAP], in1=pu_[:, 0:CAP], op=ALU.mult),
                                 reads=[f'gtmp{b}', puk_], writes=['actT'])
                    for dc in range(D // 512):
                        yb = dc % 2
                        accs = [P() for _ in range(NSL)]
                        r0 = gs[0] * 4 * 128
                        wt, wk = wload([(wd_e[r0:r0 + nft * 128, dc * 512:(dc + 1) * 512], nft, 0, 512, 512)])
                        wv = wview(wt, nft, 512)
                        S.burst([(lambda f=f, t2=t2: nc.tensor.matmul(accs[t2][0][:, :], lhsT=actT2[:, f, t2 * 128:(t2 + 1) * 128], rhs=wv[:, f, :],
                                                                      start=(f == 0), stop=(f == nft - 1))) for f in range(nft) for t2 in range(NSL)],
                                reads=['actT', wk], writes=[a[1] for a in accs])
                        for t2 in range(NSL):
                            for hf in range(2):
                                S.op('act', lambda: nc.scalar.copy(out=hnT[:, 8 + yb * 4 + t2 * 2 + hf, 256:512], in_=accs[t2][0][:, hf * 256:(hf + 1) * 256]),
                                     reads=[accs[t2][1]], writes=[f'Ydc{yb}'])
                        for s_ in range(NST):
                            po, pok = P()
                            for hf in range(2):
                                for t2 in range(NSL):
                                    S.op('pe', lambda: nc.tensor.matmul(po[:, hf * 256:(hf + 1) * 256],
                                                                        lhsT=hnT[:, 4 + t2 * 2 + s_ // 2, 256 + (s_ % 2) * 128:256 + (s_ % 2 + 1) * 128],
                                                                        rhs=hnT[:, 8 + yb * 4 + t2 * 2 + hf, 256:512], start=(t2 == 0), stop=(t2 == NSL - 1)),
                                         reads=['PTe', f'Ydc{yb}'], writes=[pok])
                            hsl = h[:, s_, dc * 512:(dc + 1) * 512]
                            S.op('dve', lambda: nc.vector.scalar_tensor_tensor(out=hsl, in0=po[:, :], scalar=gwt[:, s_, e:e + 1], in1=hsl, op0=ALU.mult, op1=ALU.add),
                                 reads=[pok, f'h{s_}', 'gwt'], writes=[f'h{s_}'])
            S.alias_barrier(['XT', 'Pe', 'PTe', 'Ydc0', 'Ydc1'], ['hnT'])

        def router():
            wtr, wkr = wload([(mwr, KT, 0, NE, 512)])
            wr = wview(wtr, KT, 512)
            for s_ in range(NST):
                pr, prk = P()
                S.burst([(lambda k=k: nc.tensor.matmul(pr[:, 0:NE], lhsT=hnT[:, k, s_ * 128:(s_ + 1) * 128], rhs=wr[:, k, 0:NE],
                                                       start=(k == 0), stop=(k == KT - 1))) for k in range(KT)],
                        reads=['hnT', wkr], writes=[prk])
                S.op('dve', lambda: nc.vector.tensor_copy(out=lg[:, s_, :], in_=pr[:, 0:NE]), reads=[prk], writes=['lg'])
                S.op('dve', lambda: nc.vector.reduce_max(out=m12[:, 0:1], in_=lg[:, s_, :], axis=AX.X), reads=['lg'], writes=['m12'])
                S.op('dve', lambda: nc.vector.tensor_scalar(out=eq1[:], in0=lg[:, s_, :], scalar1=m12[:, 0:1], scalar2=None, op0=ALU.is_equal),
                     reads=['lg', 'm12'], writes=['eq1'])
                S.op('dve', lambda: nc.vector.scalar_tensor_tensor(out=lg2[:], in0=eq1[:], scalar=-1e30, in1=lg[:, s_, :], op0=ALU.mult, op1=ALU.add),
                     reads=['eq1', 'lg'], writes=['lg2'])
                S.op('dve', lambda: nc.vector.reduce_max(out=m12[:, 1:2], in_=lg2[:], axis=AX.X), reads=['lg2'], writes=['m12'])
                S.op('dve', lambda: nc.vector.tensor_scalar(out=eq2[:], in0=lg2[:], scalar1=m12[:, 1:2], scalar2=None, op0=ALU.is_equal),
                     reads=['lg2', 'm12'], writes=['eq2'])
                S.op('dve', lambda: nc.vector.tensor_tensor(out=m12[:, 2:3], in0=m12[:, 0:1], in1=m12[:, 1:2], op=ALU.subtract),
                     reads=['m12'], writes=['m12'])
                S.op('act', lambda: nc.scalar.activation(out=m12[:, 2:3], in_=m12[:, 2:3], func=AF.Sigmoid), reads=['m12'], writes=['m12'])
                S.op('dve', lambda: nc.vector.tensor_scalar(out=m12[:, 3:4], in0=m12[:, 2:3], scalar1=-1.0, scalar2=1.0, op0=ALU.mult, op1=ALU.add),
                     reads=['m12'], writes=['m12'])
                S.op('dve', lambda: nc.vector.tensor_scalar(out=eq1[:], in0=eq1[:], scalar1=m12[:, 2:3], scalar2=None, op0=ALU.mult),
                     reads=['eq1', 'm12'], writes=['eq1'])
                S.op('dve', lambda: nc.vector.scalar_tensor_tensor(out=gwt[:, s_, :], in0=eq2[:], scalar=m12[:, 3:4], in1=eq1[:], op0=ALU.mult, op1=ALU.add),
                     reads=['eq1', 'eq2', 'm12'], writes=['gwt'])

        def ple(l, tok0):
            rmsnorm_T(4 + l)
            S.dma('sp', lambda: nc.sync.dma_start(out=ptile[:], in_=p[l * NTOK + tok0: l * NTOK + tok0 + TT, :].rearrange("(s p) c -> p s c", p=128)),
                  writes=['ptile'])
            for s_ in range(NST):
                pt, pk = P()
                for k2 in range(PL // 128):
                    S.op('pe', lambda: nc.tensor.transpose(out=pt[:, k2 * 128:(k2 + 1) * 128], in_=ptile[:, s_, k2 * 128:(k2 + 1) * 128], identity=ident),
                         reads=['ptile', 'cs'], writes=[pk])
                S.op('act', lambda: nc.scalar.copy(out=pT[:, :, s_ * 128:(s_ + 1) * 128],
                                                   in_=pt[:, 0:PL].rearrange("p (k c) -> p k c", c=128)), reads=[pk], writes=['pT'])
            for dc in range(D // 512):
                wtp, wkp = wload([(pwp[l * PL:(l + 1) * PL, dc * 512:(dc + 1) * 512], PL // 128, 0, 512, 512)])
                wvp = wview(wtp, PL // 128, 512)
                wt, wk = wload([(pwg[l * D:(l + 1) * D, dc * 512:(dc + 1) * 512], KT, 0, 512, 512)])
                wv = wview(wt, KT, 512)
                for s_ in range(NST):
                    b = s_ % 2
                    pa, pak = P()
                    proj_tm(wv, wk, 0, 512, s_, pa, pak)
                    pb, pbk = P()
                    for k2 in range(PL // 128):
                        S.op('pe', lambda: nc.tensor.matmul(pb[:, :], lhsT=pT[:, k2, s_ * 128:(s_ + 1) * 128], rhs=wvp[:, k2, :],
                                                            start=(k2 == 0), stop=(k2 == PL // 128 - 1)),
                             reads=['pT', wkp], writes=[pbk])
                    S.op('act', lambda: nc.scalar.activation(out=gtmp[b][:], in_=pa[:, :], func=AF.Sigmoid), reads=[pak], writes=[f'gtmp{b}'])
                    S.op('dve', lambda: nc.vector.tensor_tensor(out=gtmp[b][:], in0=gtmp[b][:], in1=pb[:, :], op=ALU.mult),
                         reads=[f'gtmp{b}', pbk], writes=[f'gtmp{b}'])
                    hsl = h[:, s_, dc * 512:(dc + 1) * 512]
                    S.op('dve', lambda: nc.vector.tensor_tensor(out=hsl, in0=hsl, in1=gtmp[b][:], op=ALU.add),
                         reads=[f'gtmp{b}', f'h{s_}'], writes=[f'h{s_}'])

        def dump(idx, tok0):
            if not dbg:
                return
            for s_ in range(NST):
                S.dma('sp', lambda: nc.sync.dma_start(out=dbgo[idx * NTOK + tok0 + s_ * 128: idx * NTOK + tok0 + (s_ + 1) * 128, :], in_=h[:, s_, :]),
                      reads=[f'h{s_}'], writes=['dbgo'], accum=True)

        for sq in range(NSEQ):
            for l in range(2):
                for hd in range(HH):
                    S.op('dve', lambda: nc.vector.memset(Sh[:, l, hd, :], 0.0), writes=[f"Sh{l}_{hd}"])
                for gh in range(GH):
                    S.op('dve', lambda: nc.vector.memset(Sg[:, l, gh, :], 0.0), writes=[f"Sg{l}_{gh}"])
            for ti in range(NTILE):
                tok0 = sq * T + ti * TT
                pre = ti < NPRE
                otok0 = (sq * NOWN + (ti - NPRE)) * TT
                for s_ in range(NST):
                    S.dma('sp', lambda: nc.sync.dma_start(out=h[:, s_, :], in_=x[tok0 + s_ * 128: tok0 + (s_ + 1) * 128, :]), writes=[f'h{s_}'])
                for l in range(2):
                    if pre and l == 1:
                        mixer(1, state_only=True)
                        break
                    mixer(l)
                    dump(3 * l + 0, tok0)
                    if l == 0:
                        rmsnorm_T(2)
                        swiglu(dwg, dwu, dwd, F0)
                    elif cfg.get('dense_moe', False):
                        rmsnorm_T(3)
                        router()
                        for e in range(NE):
                            swiglu(mwg[e * D:(e + 1) * D, :], mwu[e * D:(e + 1) * D, :], mwd[e * FE:(e + 1) * FE, :], FE, gate_col=e)
                    else:
                        rmsnorm_T(3, keep_tok=True)
                        router()
                        moe_sparse()
                    dump(3 * l + 1, tok0)
                    ple(l, tok0)
                    dump(3 * l + 2, tok0)
                if pre:
                    continue
                for s_ in range(NST):
                    S.op('dve', lambda: nc.vector.tensor_tensor(out=tmpN[:], in0=h[:, s_, :], in1=h[:, s_, :], op=ALU.mult),
                         reads=[f'h{s_}'], writes=['tmpN'])
                    S.op('dve', lambda: nc.vector.reduce_sum(out=ss[:, 0:1], in_=tmpN[:], axis=AX.X), reads=['tmpN'], writes=['ss'])
                    S.op('dve', lambda: nc.vector.tensor_scalar(out=ss[:, 1:2], in0=ss[:, 0:1], scalar1=1.0 / D, scalar2=EPS, op0=ALU.mult, op1=ALU.add),
                         reads=['ss'], writes=['ss'])
                    S.op('act', lambda: nc.scalar.activation(out=ss[:, 2:3], in_=ss[:, 1:2], func=AF.Sqrt), reads=['ss'], writes=['ss'])
                    S.op('dve', lambda: nc.vector.reciprocal(out=rs[:, 0:1], in_=ss[:, 2:3]), reads=['ss'], writes=['rs'])
                    S.op('dve', lambda: nc.vector.scalar_tensor_tensor(out=tmpN[:], in0=h[:, s_, :], scalar=rs[:, 0:1], in1=wfin[:], op0=ALU.mult, op1=ALU.mult),
                         reads=[f'h{s_}', 'rs', 'wfin'], writes=['tmpN'])
                    S.dma('sp', lambda: nc.sync.dma_start(out=out[otok0 + s_ * 128: otok0 + (s_ + 1) * 128, :], in_=tmpN[:]), reads=['tmpN'], writes=['out'], accum=True)
        S.wait_all('sp', ['out', 'dbgo'])
        for q in ('sp',):
            for k in range(S.NDMA):
                if S.dval[q][k] > 0:
                    nc.sync.wait_ge(S.dsem[q][k], S.dval[q][k])
        print(f"[build] instructions={S.ninst} waits={S.nwait}", flush=True)
    return nc


def core_inputs(cfg, inputs, slots):
    D = cfg['D']
    f = lambda a: np.ascontiguousarray(np.asarray(a, dtype=np.float32))
    npre = cfg.get('NPRE', 0) * TT

    def rows(a, seq, plo, phi, olo, ohi):
        a = np.asarray(a)
        pre = a[seq, plo:phi] if phi > plo else np.zeros((npre,) + a.shape[2:], np.float32)
        return np.concatenate([pre, a[seq, olo:ohi]], 0) if npre else a[seq, olo:ohi]

    x = np.concatenate([rows(inputs['x'], *sl) for sl in slots], 0)
    p = np.concatenate([np.concatenate([rows(inputs['p'][l], *sl) for sl in slots], 0) for l in range(2)], 0)
    NE = cfg['NE']
    d = {
        "x": f(x), "p": f(p),
        "w_in": f(inputs['w_in']).reshape(2 * D, -1),
        "w_out": f(inputs['w_out']).reshape(-1, D),
        "dwg": f(inputs['dense_w_gate'][0]), "dwu": f(inputs['dense_w_up'][0]), "dwd": f(inputs['dense_w_down'][0]),
        "mwr": f(inputs['moe_router'][0]),
        "mwg": f(inputs['moe_w_gate'][0]).reshape(NE * D, -1), "mwu": f(inputs['moe_w_up'][0]).reshape(NE * D, -1),
        "mwd": f(inputs['moe_w_down'][0]).reshape(-1, D),
        "pwg": f(inputs['ple_w_gate']).reshape(2 * D, D), "pwp": f(inputs['ple_w_proj']).reshape(-1, D),
        "nmix": f(inputs['norm_mix_w']), "nffn": f(inputs['norm_ffn_w']), "nple": f(inputs['norm_ple_w']),
        "nfin": f(inputs['final_norm_w']).reshape(1, D),
        "lbl": f(inputs['hgrn_lb_logits']), "hnw": f(inputs['hgrn_norm_w']), "gnw": f(inputs['gla_norm_w']),
        "gw2": f(inputs['gla_gate_w2']).reshape(-1, inputs['gla_gate_w2'].shape[-1]), "gb": f(inputs['gla_gate_b']),
        "cst": make_consts(),
    }
    return d


FULL_CFG = dict(D=2048, T=4096, NSEQ=1, HH=8, GH=4, F0=5632, FE=7168, NE=8)


def run(cfg, inputs, ncores, dbg=False, trace=False, split=1):
    B, Tfull = inputs['x'].shape[0], inputs['x'].shape[1]
    D = cfg['D']
    if split == 1:
        nseq = B // ncores
        cfg = dict(cfg, NSEQ=nseq, T=Tfull, NPRE=0)
        slots = [[(c * nseq + i, 0, 0, 0, Tfull) for i in range(nseq)] for c in range(ncores)]
    else:
        assert ncores == 2 * B
        hl = Tfull // 2
        cfg = dict(cfg, NSEQ=1, T=Tfull, NPRE=hl // TT)
        slots = [[(c // 2, 0, hl * (c % 2), hl * (c % 2), hl * (c % 2) + hl)] for c in range(ncores)]
    nc = build_program(cfg, dbg=dbg)
    in_maps = [core_inputs(cfg, inputs, slots[c]) for c in range(ncores)]
    res = run_bass_kernel_spmd(nc, in_maps, core_ids=list(range(ncores)), trace=trace)
    o = np.zeros((B, Tfull, D), np.float32)
    for c in range(ncores):
        r = res.results[c]["out"]
        off = 0
        for (sq, plo, phi, olo, ohi) in slots[c]:
            o[sq, olo:ohi] = r[off:off + (ohi - olo)]
            off += ohi - olo
    if dbg:
        return o, [r["dbg"] for r in res.results], res
    return o


def kernel(**inputs):
    return run(FULL_CFG, inputs, ncores=8, split=2)
```
